# Optimizing a Trainium2 kernel written in Bass

```python
import jax
import jax.numpy as jnp
from jax import lax
import numpy as np

D_MODEL = 4096
BATCH = 4
SEQ = 4096
DEPTH = 2

HEAD_DIM = 128
ROT_DIM = HEAD_DIM // 4
ROPE_THETA = 500000.0
NORM_EPS = 1e-6
MASK_VALUE = -1e30

DIL_GROUPS = ((128, 1), (512, 4), (2048, 16))
DIL_HEADS_PER_GROUP = 4
DIL_HEADS = len(DIL_GROUPS) * DIL_HEADS_PER_GROUP
DIL_WIDTH = DIL_HEADS * HEAD_DIM
DIL_OUT = DIL_HEADS_PER_GROUP * HEAD_DIM
MLA_HEADS = 8
MLA_Q_LORA = 1536
MLA_KV_LORA = 512
MLA_NOPE = 128
MLA_ROPE = 64
MLA_V = 128
MLA_QBLOCK = 128
MOBA_HEADS = 8
MOBA_BLOCK = 256
MOBA_TOPK = 3
MOBA_QCHUNK = 32
DSA_HEADS = 8
DSA_TOPK = 256
IDX_HEADS = 32
IDX_DIM = 64
IDX_ROT = IDX_DIM // 4
DSA_QCHUNK = 128

N_BRANCH = 4
IN_SIZES = (DIL_WIDTH, DIL_WIDTH, DIL_WIDTH,
            MLA_Q_LORA, MLA_KV_LORA, MLA_ROPE,
            MOBA_HEADS * HEAD_DIM, MOBA_HEADS * HEAD_DIM, MOBA_HEADS * HEAD_DIM,
            DSA_HEADS * HEAD_DIM, HEAD_DIM, HEAD_DIM, IDX_HEADS * IDX_DIM, IDX_DIM, IDX_HEADS,
            N_BRANCH * D_MODEL)
D_IN = sum(IN_SIZES)
IN_SPLITS = tuple(sum(IN_SIZES[:i + 1]) for i in range(len(IN_SIZES) - 1))
BRANCH_SIZES = (DIL_OUT, MLA_HEADS * MLA_V, MOBA_HEADS * HEAD_DIM, DSA_HEADS * HEAD_DIM)
D_BRANCH = sum(BRANCH_SIZES)
BRANCH_SPLITS = tuple(sum(BRANCH_SIZES[:i + 1]) for i in range(len(BRANCH_SIZES) - 1))

D_FF_DENSE = 14336
N_EXPERTS = 8
MOE_TOP_K = 2
D_FF_EXPERT = 3072
MOE_ROWS = 256
N_MOD = 6

kernel_name = 'hybrid_gated_sparse_attn_moe_decoder'


def rms_norm(x, gain):
    xf = x.astype(jnp.float32)
    y = xf * lax.rsqrt(jnp.mean(xf * xf, axis=-1, keepdims=True) + NORM_EPS)
    return (y * gain.astype(jnp.float32)).astype(x.dtype)


def rotary(x, positions, rot_dim):
    half = rot_dim // 2
    freqs = ROPE_THETA ** (-jnp.arange(half, dtype=jnp.float32) * (2.0 / rot_dim))
    ang = positions.astype(jnp.float32)[..., None] * freqs
    ang = ang.reshape(ang.shape[:2] + (1,) * (x.ndim - 3) + (half,))
    cos, sin = jnp.cos(ang), jnp.sin(ang)
    x1 = x[..., :half].astype(jnp.float32)
    x2 = x[..., half:rot_dim].astype(jnp.float32)
    rot = jnp.concatenate([x1 * cos - x2 * sin, x2 * cos + x1 * sin], axis=-1).astype(x.dtype)
    return jnp.concatenate([rot, x[..., rot_dim:]], axis=-1)


def dilated_window_attention(q, k, v, window, dilation):
    B, S, Hg, hd = q.shape
    n = S // dilation
    wsub = window // dilation
    nb = -(-n // wsub)
    npad = nb * wsub

    def to_blocks(t):
        t = t.reshape(B, n, dilation, Hg, hd).transpose(0, 2, 3, 1, 4)
        t = jnp.pad(t, ((0, 0), (0, 0), (0, 0), (0, npad - n), (0, 0)))
        return t.reshape(B, dilation, Hg, nb, wsub, hd)

    def with_prev(t):
        prev = jnp.pad(t[:, :, :, :-1], ((0, 0), (0, 0), (0, 0), (1, 0), (0, 0), (0, 0)))
        return jnp.concatenate([prev, t], axis=4)

    qb = to_blocks(q)
    kw = with_prev(to_blocks(k))
    vw = with_prev(to_blocks(v))
    s = jnp.einsum('brhnqd,brhnkd->brhnqk', qb, kw, preferred_element_type=jnp.float32) * (hd ** -0.5)
    ki = jnp.arange(2 * wsub)[None, :]
    dist = (jnp.arange(wsub)[:, None] + wsub) - ki
    band = (dist >= 0) & (dist <= wsub)
    has_prev = (jnp.arange(nb) > 0)[:, None, None] | (ki >= wsub)[None]
    s = jnp.where(band[None] & has_prev, s, MASK_VALUE)
    lse = jax.nn.logsumexp(s, axis=-1)
    p = jnp.exp(s - lse[..., None])
    o = jnp.einsum('brhnqk,brhnkd->brhnqd', p.astype(v.dtype), vw)
    o = o.reshape(B, dilation, Hg, npad, hd)[:, :, :, :n].transpose(0, 3, 1, 2, 4).reshape(B, S, Hg, hd)
    lse = lse.reshape(B, dilation, Hg, npad)[..., :n].transpose(0, 3, 1, 2).reshape(B, S, Hg)
    return o, lse


def dilated_mixture(q, k, v, positions):
    B, S, _ = q.shape
    q = rotary(q.reshape(B, S, DIL_HEADS, HEAD_DIM), positions, ROT_DIM)
    k = rotary(k.reshape(B, S, DIL_HEADS, HEAD_DIM), positions, ROT_DIM)
    v = v.reshape(B, S, DIL_HEADS, HEAD_DIM)
    outs, lses = [], []
    for g, (window, dilation) in enumerate(DIL_GROUPS):
        hs = slice(g * DIL_HEADS_PER_GROUP, (g + 1) * DIL_HEADS_PER_GROUP)
        o_g, lse_g = dilated_window_attention(q[:, :, hs], k[:, :, hs], v[:, :, hs], window, dilation)
        outs.append(o_g)
        lses.append(lse_g)
    w = jax.nn.softmax(jnp.stack(lses), axis=0)
    o = jnp.einsum('gbsh,gbshd->bshd', w, jnp.stack(outs).astype(jnp.float32))
    return o.astype(q.dtype).reshape(B, S, DIL_OUT)


def mla_attention(cq, ckv, k_rope_in, positions, q_norm, w_q_up, kv_norm, w_kv_up):
    B, S, _ = cq.shape
    H = MLA_HEADS
    q = (rms_norm(cq, q_norm) @ w_q_up).reshape(B, S, H, MLA_NOPE + MLA_ROPE)
    q_nope = q[..., :MLA_NOPE]
    q_pe = rotary(q[..., MLA_NOPE:], positions, MLA_ROPE)
    kv = (rms_norm(ckv, kv_norm) @ w_kv_up).reshape(B, S, H, MLA_NOPE + MLA_V)
    k_nope, v = kv[..., :MLA_NOPE], kv[..., MLA_NOPE:]
    k_pe = rotary(k_rope_in, positions, MLA_ROPE)
    nq = S // MLA_QBLOCK
    scale = (MLA_NOPE + MLA_ROPE) ** -0.5
    kpos = jnp.arange(S)

    def block(args):
        qn, qp, i = args
        qpos = i * MLA_QBLOCK + jnp.arange(MLA_QBLOCK)
        s = (jnp.einsum('bqhd,bkhd->bhqk', qn, k_nope, preferred_element_type=jnp.float32)
             + jnp.einsum('bqhr,bkr->bhqk', qp, k_pe, preferred_element_type=jnp.float32)) * scale
        s = jnp.where(kpos[None, :] <= qpos[:, None], s, MASK_VALUE)
        p = jax.nn.softmax(s, axis=-1)
        return jnp.einsum('bhqk,bkhd->bqhd', p.astype(v.dtype), v)

    def to_blocks(t):
        return t.reshape((B, nq, MLA_QBLOCK) + t.shape[2:]).swapaxes(0, 1)

    o = lax.map(block, (to_blocks(q_nope), to_blocks(q_pe), jnp.arange(nq)))
    return o.swapaxes(0, 1).reshape(B, S, H * MLA_V)


def moba_attention(q, k, v, positions):
    B, S, _ = q.shape
    H, hd = MOBA_HEADS, HEAD_DIM
    q = rotary(q.reshape(B, S, H, hd), positions, ROT_DIM)
    k = rotary(k.reshape(B, S, H, hd), positions, ROT_DIM)
    v = v.reshape(B, S, H, hd)
    nb = -(-S // MOBA_BLOCK)
    sp = nb * MOBA_BLOCK
    pad = ((0, 0), (0, sp - S), (0, 0), (0, 0))
    q, k, v = jnp.pad(q, pad), jnp.pad(k, pad), jnp.pad(v, pad)
    kb = k.reshape(B, nb, MOBA_BLOCK, H, hd).transpose(0, 3, 1, 2, 4)
    vb = v.reshape(B, nb, MOBA_BLOCK, H, hd).transpose(0, 3, 1, 2, 4)
    k_mean = jnp.mean(kb.astype(jnp.float32), axis=3)
    n_sel = min(MOBA_TOPK, nb)
    scale = hd ** -0.5
    bi = jnp.arange(B)[:, None, None, None]
    hi = jnp.arange(H)[None, :, None, None]
    blk_ids = jnp.arange(nb)

    def chunk(args):
        qc, ci = args
        q0 = ci * MOBA_QCHUNK
        qpos = q0 + jnp.arange(MOBA_QCHUNK)
        own = q0 // MOBA_BLOCK
        gate = jnp.einsum('bqhd,bhnd->bhqn', qc.astype(jnp.float32), k_mean)
        gate = jnp.where(blk_ids < own, gate, MASK_VALUE)
        _, sel = lax.top_k(gate, n_sel)
        sel_ok = sel < own
        k_sel = kb[bi, hi, sel]
        v_sel = vb[bi, hi, sel]
        s_sel = jnp.einsum('bqhd,bhqjkd->bhqjk', qc, k_sel, preferred_element_type=jnp.float32) * scale
        s_sel = jnp.where(sel_ok[..., None], s_sel, MASK_VALUE).reshape(B, H, MOBA_QCHUNK, n_sel * MOBA_BLOCK)
        k_own = lax.dynamic_index_in_dim(kb, own, axis=2, keepdims=False)
        v_own = lax.dynamic_index_in_dim(vb, own, axis=2, keepdims=False)
        kpos = own * MOBA_BLOCK + jnp.arange(MOBA_BLOCK)
        s_own = jnp.einsum('bqhd,bhkd->bhqk', qc, k_own, preferred_element_type=jnp.float32) * scale
        s_own = jnp.where(kpos[None, :] <= qpos[:, None], s_own, MASK_VALUE)
        p = jax.nn.softmax(jnp.concatenate([s_sel, s_own], axis=-1), axis=-1)
        p_sel = p[..., :n_sel * MOBA_BLOCK].reshape(B, H, MOBA_QCHUNK, n_sel, MOBA_BLOCK).astype(v.dtype)
        p_own = p[..., n_sel * MOBA_BLOCK:].astype(v.dtype)
        return (jnp.einsum('bhqjk,bhqjkd->bqhd', p_sel, v_sel)
                + jnp.einsum('bhqk,bhkd->bqhd', p_own, v_own))

    nch = sp // MOBA_QCHUNK
    qch = q.reshape(B, nch, MOBA_QCHUNK, H, hd).swapaxes(0, 1)
    o = lax.map(chunk, (qch, jnp.arange(nch)))
    return o.swapaxes(0, 1).reshape(B, sp, H * hd)[:, :S]


def dsa_attention(q, k, v, iq, ik, iw, positions):
    B, S, _ = q.shape
    H, hd = DSA_HEADS, HEAD_DIM
    q = rotary(q.reshape(B, S, H, hd), positions, ROT_DIM)
    k = rotary(k, positions, ROT_DIM)
    iq = rotary(iq.reshape(B, S, IDX_HEADS, IDX_DIM), positions, IDX_ROT)
    ik = rotary(ik, positions, IDX_ROT)
    iw = iw.astype(jnp.float32) * (IDX_HEADS ** -0.5)
    n_keep = min(DSA_TOPK, S // 4)
    nch = S // DSA_QCHUNK
    kpos = jnp.arange(S)
    scale = hd ** -0.5
    gather = jax.vmap(lambda t, idx: t[idx])

    def chunk(args):
        qc, iqc, iwc, ci = args
        qpos = ci * DSA_QCHUNK + jnp.arange(DSA_QCHUNK)
        rel = jax.nn.relu(jnp.einsum('bqhd,bsd->bqhs', iqc, ik, preferred_element_type=jnp.float32) * (IDX_DIM ** -0.5))
        score = jnp.einsum('bqhs,bqh->bqs', rel, iwc)
        score = jnp.where(kpos[None, :] <= qpos[:, None], score, -jnp.inf)
        _, sel = lax.top_k(score, n_keep)
        ok = sel <= qpos[None, :, None]
        k_sel = gather(k, sel)
        v_sel = gather(v, sel)
        s = jnp.einsum('bqhd,bqkd->bhqk', qc, k_sel, preferred_element_type=jnp.float32) * scale
        s = jnp.where(ok[:, None], s, MASK_VALUE)
        p = jax.nn.softmax(s, axis=-1)
        return jnp.einsum('bhqk,bqkd->bqhd', p.astype(v.dtype), v_sel)

    def split(t):
        return t.reshape((B, nch, DSA_QCHUNK) + t.shape[2:]).swapaxes(0, 1)

    o = lax.map(chunk, (split(q), split(iq), split(iw), jnp.arange(nch)))
    return o.swapaxes(0, 1).reshape(B, S, H * hd)


def hybrid_token_mixer(h, positions, w_in, mla_q_norm, mla_q_up, mla_kv_norm, mla_kv_up, w_branch, w_out):
    B, S, D = h.shape
    (a_q, a_k, a_v, b_cq, b_ckv, b_kr, c_q, c_k, c_v,
     d_q, d_k, d_v, d_iq, d_ik, d_iw, g) = jnp.split(h @ w_in, IN_SPLITS, axis=-1)
    o_a = dilated_mixture(a_q, a_k, a_v, positions)
    o_b = mla_attention(b_cq, b_ckv, b_kr, positions, mla_q_norm, mla_q_up, mla_kv_norm, mla_kv_up)
    o_c = moba_attention(c_q, c_k, c_v, positions)
    o_d = dsa_attention(d_q, d_k, d_v, d_iq, d_ik, d_iw, positions)
    p_a, p_b, p_c, p_d = jnp.split(w_branch, BRANCH_SPLITS, axis=0)
    gates = jax.nn.sigmoid(g.astype(jnp.float32)).astype(h.dtype).reshape(B, S, N_BRANCH, D)
    y = (gates[:, :, 0] * (o_a @ p_a) + gates[:, :, 1] * (o_b @ p_b)
         + gates[:, :, 2] * (o_c @ p_c) + gates[:, :, 3] * (o_d @ p_d))
    return y @ w_out


def swiglu(h, w_gate, w_up, w_down):
    return (jax.nn.silu(h @ w_gate) * (h @ w_up)) @ w_down


def moe_swiglu(h, w_router, w_gate, w_up, w_down):
    B, S, D = h.shape
    T = B * S
    A = T * MOE_TOP_K
    ht = h.reshape(T, D)
    logits = jnp.matmul(ht, w_router, preferred_element_type=jnp.float32)
    top_logits, top_idx = lax.top_k(logits, MOE_TOP_K)
    top_w = jax.nn.softmax(top_logits, axis=-1)
    e_flat = top_idx.reshape(A)
    tok_flat = jnp.repeat(jnp.arange(T, dtype=jnp.int32), MOE_TOP_K)
    w_flat = top_w.reshape(A)
    order = jnp.argsort(e_flat)
    e_s, tok_s, w_s = e_flat[order], tok_flat[order], w_flat[order]
    counts = jnp.bincount(e_flat, length=N_EXPERTS)
    starts = jnp.cumsum(counts) - counts
    padded = (counts + MOE_ROWS - 1) // MOE_ROWS * MOE_ROWS
    pends = jnp.cumsum(padded)
    pstarts = pends - padded
    dest = pstarts[e_s] + jnp.arange(A) - starts[e_s]
    n_blocks = -(-A // MOE_ROWS) + N_EXPERTS
    P = n_blocks * MOE_ROWS
    x_buf = jnp.zeros((P, D), h.dtype).at[dest].set(ht[tok_s])
    tok_buf = jnp.full((P,), T, jnp.int32).at[dest].set(tok_s)
    w_buf = jnp.zeros((P,), jnp.float32).at[dest].set(w_s)
    block_expert = jnp.minimum(jnp.searchsorted(pends, jnp.arange(n_blocks) * MOE_ROWS, side='right'), N_EXPERTS - 1)

    def expert_block(args):
        xb, e = args
        return swiglu(xb, w_gate[e], w_up[e], w_down[e])

    y_buf = lax.map(expert_block, (x_buf.reshape(n_blocks, MOE_ROWS, D), block_expert)).reshape(P, D)
    out = jnp.zeros((T, D), h.dtype).at[tok_buf].add(y_buf * w_buf[:, None].astype(h.dtype), mode='drop')
    return out.reshape(B, S, D)


def setup_inputs(seed: int = 0) -> dict:
    key = jax.random.key(seed)
    keys = iter(jax.random.split(key, 64))
    D = D_MODEL

    def normal(shape, std):
        return jax.random.normal(next(keys), shape, jnp.float32) * std

    def gain(n):
        return 1.0 + normal((n,), 0.02)

    inputs = {}
    inputs['x'] = normal((BATCH, SEQ, D), 1.0)
    inputs['c'] = normal((BATCH, D), 1.0)
    offsets = jax.random.randint(next(keys), (BATCH, 1), 0, 1024, dtype=jnp.int32)
    inputs['positions'] = offsets + jnp.arange(SEQ, dtype=jnp.int32)[None, :]
    inputs['w_ada'] = normal((D, N_MOD * D), 0.5 * D ** -0.5)
    inputs['b_ada'] = normal((N_MOD * D,), 0.02)
    for layer in range(DEPTH):
        inputs[f'ada_table_{layer}'] = normal((N_MOD, D), 0.1)
        inputs[f'mix_norm_{layer}'] = gain(D)
        inputs[f'w_in_{layer}'] = normal((D, D_IN), D ** -0.5)
        inputs[f'mla_q_norm_{layer}'] = gain(MLA_Q_LORA)
        inputs[f'mla_q_up_{layer}'] = normal((MLA_Q_LORA, MLA_HEADS * (MLA_NOPE + MLA_ROPE)), MLA_Q_LORA ** -0.5)
        inputs[f'mla_kv_norm_{layer}'] = gain(MLA_KV_LORA)
        inputs[f'mla_kv_up_{layer}'] = normal((MLA_KV_LORA, MLA_HEADS * (MLA_NOPE + MLA_V)), MLA_KV_LORA ** -0.5)
        inputs[f'w_branch_{layer}'] = jnp.concatenate([normal((n, D), n ** -0.5) for n in BRANCH_SIZES], axis=0)
        inputs[f'w_out_{layer}'] = normal((D, D), D ** -0.5)
        inputs[f'ffn_norm_{layer}'] = gain(D)
        if layer % 2 == 0:
            inputs[f'ffn_gate_{layer}'] = normal((D, D_FF_DENSE), D ** -0.5)
            inputs[f'ffn_up_{layer}'] = normal((D, D_FF_DENSE), D ** -0.5)
            inputs[f'ffn_down_{layer}'] = normal((D_FF_DENSE, D), D_FF_DENSE ** -0.5)
        else:
            inputs[f'router_{layer}'] = normal((D, N_EXPERTS), D ** -0.5)
            inputs[f'expert_gate_{layer}'] = normal((N_EXPERTS, D, D_FF_EXPERT), D ** -0.5)
            inputs[f'expert_up_{layer}'] = normal((N_EXPERTS, D, D_FF_EXPERT), D ** -0.5)
            inputs[f'expert_down_{layer}'] = normal((N_EXPERTS, D_FF_EXPERT, D), D_FF_EXPERT ** -0.5)
    inputs['final_norm'] = gain(D)
    return inputs


def reference(x, c, positions, w_ada, b_ada,
              ada_table_0, mix_norm_0, w_in_0, mla_q_norm_0, mla_q_up_0, mla_kv_norm_0, mla_kv_up_0,
              w_branch_0, w_out_0, ffn_norm_0, ffn_gate_0, ffn_up_0, ffn_down_0,
              ada_table_1, mix_norm_1, w_in_1, mla_q_norm_1, mla_q_up_1, mla_kv_norm_1, mla_kv_up_1,
              w_branch_1, w_out_1, ffn_norm_1, router_1, expert_gate_1, expert_up_1, expert_down_1,
              final_norm):
    B, S, D = x.shape
    mod_shared = (jax.nn.silu(c) @ w_ada + b_ada).reshape(B, N_MOD, D)
    mixer_params = (
        (ada_table_0, mix_norm_0, w_in_0, mla_q_norm_0, mla_q_up_0, mla_kv_norm_0, mla_kv_up_0, w_branch_0, w_out_0),
        (ada_table_1, mix_norm_1, w_in_1, mla_q_norm_1, mla_q_up_1, mla_kv_norm_1, mla_kv_up_1, w_branch_1, w_out_1),
    )
    ffn_params = (
        (ffn_norm_0, (ffn_gate_0, ffn_up_0, ffn_down_0)),
        (ffn_norm_1, (router_1, expert_gate_1, expert_up_1, expert_down_1)),
    )
    for layer in range(DEPTH):
        ada_table, mix_norm, w_in, qn, qu, kvn, kvu, w_branch, w_out = mixer_params[layer]
        ffn_norm, ffn_w = ffn_params[layer]
        mod = mod_shared + ada_table[None]
        shift_m, scale_m, gate_m, shift_f, scale_f, gate_f = [mod[:, i, None, :] for i in range(N_MOD)]
        h = rms_norm(x, mix_norm) * (1 + scale_m) + shift_m
        x = x + gate_m * hybrid_token_mixer(h, positions, w_in, qn, qu, kvn, kvu, w_branch, w_out)
        h = rms_norm(x, ffn_norm) * (1 + scale_f) + shift_f
        ffn_out = swiglu(h, *ffn_w) if layer % 2 == 0 else moe_swiglu(h, *ffn_w)
        x = x + gate_f * ffn_out
    return rms_norm(x, final_norm)
```

```python
import numpy as np
from contextlib import ExitStack
import concourse.bass as bass
import concourse.mybir as mybir
from concourse.bass_utils import run_bass_kernel_spmd

F32 = mybir.dt.float32
BF16 = mybir.dt.bfloat16
I32 = mybir.dt.int32
AF = mybir.ActivationFunctionType
ALU = mybir.AluOpType
AX = mybir.AxisListType

D = 4096
T = 4096
KC = D // 128
NT = T // 128
EPS = 1e-6
THETA = 500000.0
NEG = -1e30
OFF = {}
_sizes = [("a_q", 1536), ("a_k", 1536), ("a_v", 1536), ("b_cq", 1536), ("b_ckv", 512), ("b_kr", 64),
          ("c_q", 1024), ("c_k", 1024), ("c_v", 1024), ("d_q", 1024), ("d_k", 128), ("d_v", 128),
          ("d_iq", 2048), ("d_ik", 64), ("d_iw", 32), ("g", 16384)]
_o = 0
for _n, _s in _sizes:
    OFF[_n] = (_o, _s)
    _o += _s
D_IN = _o
YF_ROWS = {}
_o = 0
for _n, _s in [("a_q", 1536), ("a_k", 1536), ("b_cq", 1536), ("b_ckv", 512), ("kr2", 128), ("c_q", 1024), ("c_k", 1024),
               ("d_q", 1024), ("d_k", 128), ("d_iq", 2048), ("ik2", 128)]:
    YF_ROWS[_n] = _o
    _o += _s
YF_N = _o
D_FF = 14336
N_EXP = 8
D_FFE = 3072


class Sync:
    def __init__(self, nc, stack):
        self.nc = nc
        self.stack = stack
        self.engs = {"pe": nc.tensor, "act": nc.scalar, "dve": nc.vector, "pool": nc.gpsimd, "sp": nc.sync}
        self.sem = {}
        self.cnt = {}
        for e in ("pe", "act", "dve", "pool"):
            self.sem[e] = stack.enter_context(nc.semaphore("s_" + e))
            self.cnt[e] = 0
        self.seen = {e: {} for e in self.engs}
        self.last_write = {}
        self.readers = {}
        self.nsem = 0
        self.ninst = 0
        self.keysem = {}
        self.freesems = []

    def _wait(self, eng, tok):
        if tok is None:
            return
        name, val = tok
        if self.seen[eng].get(name, 0) >= val:
            return
        self.engs[eng].wait_ge(self.sem[name], val)
        self.seen[eng][name] = val

    def _record(self, tok, reads, writes):
        for k in writes:
            self.last_write[k] = tok
            self.readers[k] = {}
        for k in reads:
            d = self.readers.setdefault(k, {})
            if d.get(tok[0], 0) < tok[1]:
                d[tok[0]] = tok[1]

    def op(self, eng, fn, reads=(), writes=(), sig=True):
        selfsync = eng != "pe"

        def need(t):
            if t is None:
                return False
            if t[0] == eng:
                return selfsync and t[1] <= self.cnt[eng]
            return True
        for k in reads:
            t = self.last_write.get(k)
            if need(t):
                self._wait(eng, t)
        for k in writes:
            t = self.last_write.get(k)
            if need(t):
                self._wait(eng, t)
            for tok in self.readers.get(k, {}).items():
                if need(tok):
                    self._wait(eng, tok)
        inst = fn()
        self.ninst += 1
        if sig:
            self.cnt[eng] += 1
            inst.then_inc(self.sem[eng], 1)
            tok = (eng, self.cnt[eng])
        else:
            tok = (eng, self.cnt[eng] + 1)
        self._record(tok, reads, writes)
        return tok

    def dma(self, q, out, in_, reads=(), writes=(), nowaw=False, **kw):
        key = writes[0]
        if key not in self.keysem:
            if self.freesems:
                self.keysem[key] = self.freesems.pop()
            else:
                nm = "dsem%d" % self.nsem
                self.sem[nm] = self.stack.enter_context(self.nc.semaphore("sd%d" % self.nsem))
                self.nsem += 1
                self.cnt[nm] = 0
                self.keysem[key] = nm
        name = self.keysem[key]
        for k in reads:
            self._wait(q, self.last_write.get(k))
        for k in writes:
            if not nowaw:
                self._wait(q, self.last_write.get(k))
            for tok in self.readers.get(k, {}).items():
                self._wait(q, tok)
        inst = self.engs[q].dma_start(out=out, in_=in_, **kw)
        self.ninst += 1
        self.cnt[name] += 16
        inst.then_inc(self.sem[name], 16)
        tok = (name, self.cnt[name])
        self._record(tok, reads, writes)
        return tok

    def barrier(self):
        for e in self.engs:
            for name, c in self.cnt.items():
                if c > 0:
                    self._wait(e, (name, c))
        self.freesems.extend(self.keysem.values())
        self.keysem = {}
        self.last_write = {}
        self.readers = {}

    def wait_keys(self, eng, keys):
        for k in keys:
            self._wait(eng, self.last_write.get(k))


class Ctx:
    pass


def build(layers=(0, 1), stop=None, taps=()):
    nc = bass.Bass("TRN2", target_bir_lowering=False)
    top = ExitStack()
    g = Ctx()
    g.nc = nc
    with top:
        S = Sync(nc, top)
        g.S = S
        din = lambda n, s, dt=F32: nc.dram_tensor(n, list(s), dt, kind="ExternalInput").ap()
        dscr = lambda n, s, dt=F32: nc.dram_tensor(n, list(s), dt, kind="Internal").ap()
        shapes = {"xT": ([D, T], F32), "cc": ([128, KC], F32), "pos": ([1, T], I32), "w_ada": ([D, 6 * D], F32),
                  "b_ada": ([128, 6 * KC], F32), "freqs": ([128, 3], F32), "perm": ([3, 128, 128], F32),
                  "final_norm": ([128, KC], F32)}
        for l in (0, 1):
            L = str(l)
            shapes.update({"ada_table_" + L: ([128, 6 * KC], F32), "mix_norm_" + L: ([128, KC], F32), "w_in_" + L: ([D, D_IN], F32),
                           "w_kr2_" + L: ([D, 128], F32), "w_ik2_" + L: ([D, 128], F32), "mla_q_norm_" + L: ([128, 12], F32),
                           "mla_q_up_" + L: ([1536, 1536], F32), "mla_kv_norm_" + L: ([128, 4], F32), "mla_kv_up_" + L: ([512, 2048], F32),
                           "w_branch_" + L: ([3584, D], F32), "w_out_" + L: ([D, D], F32), "ffn_norm_" + L: ([128, KC], F32)})
        shapes.update({"ffn_gate_0": ([D, D_FF], F32), "ffn_up_0": ([D, D_FF], F32), "ffn_down_0": ([D_FF, D], F32),
                       "router_1": ([D, N_EXP], F32), "expert_gate_1": ([N_EXP, D, D_FFE], F32), "expert_up_1": ([N_EXP, D, D_FFE], F32),
                       "expert_down_1": ([N_EXP * D_FFE, D], F32)})

        class LazyIn(dict):
            def __missing__(self, k):
                shp, dt = shapes[k]
                v = nc.dram_tensor(k, list(shp), dt, kind="ExternalInput").ap()
                self[k] = v
                return v
        I = LazyIn()
        g.I = I
        outT = nc.dram_tensor("outT", [D, T], F32, kind="ExternalOutput").ap()
        R = {}
        R["YF"] = dscr("YF", [YF_N, T])
        R["G"] = dscr("G", [4 * D, T])
        R["VA"] = dscr("VA", [T, 1536], BF16)
        R["VC"] = dscr("VC", [T, 1024], BF16)
        R["VD"] = dscr("VD", [T, 128], BF16)
        R["IW"] = dscr("IW", [T, 32])
        R["VB"] = dscr("VB", [T, 1024], BF16)
        R["QB"] = dscr("QB", [1536, T], BF16)
        R["KB"] = dscr("KB", [1024, T], BF16)
        R["KPE"] = dscr("KPE", [128, T], BF16)
        R["IQR"] = dscr("IQR", [2048, T])
        R["OT"] = dscr("OT", [3584, T], BF16)
        R["YT"] = dscr("YT", [D, T], BF16)
        R["HID"] = dscr("HID", [N_EXP * D_FFE, T], BF16)
        R["CS"] = dscr("CS", [3 * 2 * 128, T])
        R["RW"] = dscr("RW", [N_EXP, T])
        R["X1"] = dscr("X1", [D, T])
        R["X2"] = dscr("X2", [D, T])
        R["X3"] = dscr("X3", [D, T])
        R["X4"] = dscr("X4", [D, T])
        g.R = R
        tap_out = {}
        sb_taps = [(src, oname) for (src, oname) in taps if src.startswith("@")]
        taps = [(src, oname) for (src, oname) in taps if not src.startswith("@")]
        for (src, oname) in taps:
            a = R[src]
            tap_out[oname] = (src, nc.dram_tensor(oname, list(a.shape), a.dtype, kind="ExternalOutput").ap())
        g.uid = 0

        def sbt(st, n, s, dt=F32):
            g.uid += 1
            return st.enter_context(nc.sbuf_tensor("t%d_%s" % (g.uid, n), list(s), dt))
        def pst(st, n, s, dt=F32):
            g.uid += 1
            full = [128, 512] if dt == F32 else [128, 1024]
            return st.enter_context(nc.psum_tensor("t%d_%s" % (g.uid, n), full, dt))
        g.sbt, g.pst = sbt, pst
        g.ident = sbt(top, "ident", [128, 128], BF16)
        g.identf = sbt(top, "identf", [128, 128])
        g.ones = sbt(top, "ones", [128, 128], BF16)
        g.onesf = sbt(top, "onesf", [128, 128])
        g.mk_le = sbt(top, "mk_le", [128, 128], BF16)
        g.mk_ge = sbt(top, "mk_ge", [128, 128], BF16)
        g.negm = sbt(top, "negm", [128, 128])
        g.permT = sbt(top, "permT", [128, 3, 128])
        g.mod = {l: sbt(top, "mod%d" % l, [128, 6 * KC]) for l in layers}
        g.AB = {l: sbt(top, "AB%d" % l, [128, 4 * KC]) for l in layers}
        g.epsc = sbt(top, "epsc", [128, 1])
        g.fnorm = sbt(top, "fnorm", [128, KC])

        phases = []

        def phase(name, fn):
            phases.append((name, fn))

        phase("setup", lambda: ph_setup(g, layers))
        xs = [I["xT"], R["X1"], R["X2"], R["X3"], R["X4"]]
        for l in layers:
            phase("in%d" % l, lambda l=l: ph_mixer_in(g, l, xs[2 * l]))
            phase("mla%d" % l, lambda l=l: ph_mla(g, l))
            phase("moba%d" % l, lambda l=l: ph_moba(g, l))
            phase("dsa%d" % l, lambda l=l: ph_dsa(g, l))
            phase("dil%d" % l, lambda l=l: ph_dil(g, l))
            phase("branch%d" % l, lambda l=l: ph_branch(g, l))
            phase("wout%d" % l, lambda l=l: ph_wout(g, l, xs[2 * l], xs[2 * l + 1]))
            phase("ffn%d" % l, lambda l=l: ph_ffn(g, l, xs[2 * l + 1], xs[2 * l + 2]))
        phase("final", lambda: ph_final(g, xs[2 * len(layers)], outT))
        for name, fn in phases:
            fn()
            S.barrier()
            if stop == name:
                break
        for (src, oname) in sb_taps:
            tl = {"@mod0": g.mod.get(0), "@AB0": g.AB.get(0), "@mod1": g.mod.get(1), "@AB1": g.AB.get(1)}[src]
            o_ap = nc.dram_tensor(oname, list(tl.shape), F32, kind="ExternalOutput").ap()
            S.dma("sp", o_ap[:, :], tl[:], writes=["tap_" + oname])
        for oname, (src, dst) in tap_out.items():
            a = R[src]
            rows = a.shape[0]
            step = 128
            for r0 in range(0, rows, step):
                r1 = min(rows, r0 + step)
                S.dma("sp", dst[r0:r1, :], a[r0:r1, :], reads=[src], writes=["tap_" + oname], nowaw=True)
        S.barrier()
    g.ninst = S.ninst
    return nc, g


def ph_setup(g, layers):
    nc, S, I, R = g.nc, g.S, g.I, g.R
    with ExitStack() as st:
        S.op("pool", lambda: nc.gpsimd.memset(g.ident[:], 0.0), writes=["ident"])
        S.op("pool", lambda: nc.gpsimd.affine_select(g.ident[:], g.ident[:], pattern=[[-1, 128]], compare_op=ALU.not_equal,
                                                      fill=1.0, base=0, channel_multiplier=1), reads=["ident"], writes=["ident"])
        S.op("dve", lambda: nc.vector.tensor_copy(g.identf[:], g.ident[:]), reads=["ident"], writes=["identf"])
        S.op("pool", lambda: nc.gpsimd.memset(g.ones[:], 1.0), writes=["ones"])
        S.op("pool", lambda: nc.gpsimd.memset(g.onesf[:], 1.0), writes=["onesf"])
        S.op("pool", lambda: nc.gpsimd.memset(g.epsc[:], EPS), writes=["epsc"])
        S.op("pool", lambda: nc.gpsimd.memset(g.mk_le[:], 1.0), writes=["mk_le"])
        S.op("pool", lambda: nc.gpsimd.affine_select(g.mk_le[:], g.mk_le[:], pattern=[[1, 128]], compare_op=ALU.is_ge,
                                                      fill=0.0, base=0, channel_multiplier=-1), reads=["mk_le"], writes=["mk_le"])
        S.op("pool", lambda: nc.gpsimd.memset(g.mk_ge[:], 1.0), writes=["mk_ge"])
        S.op("pool", lambda: nc.gpsimd.affine_select(g.mk_ge[:], g.mk_ge[:], pattern=[[-1, 128]], compare_op=ALU.is_ge,
                                                      fill=0.0, base=0, channel_multiplier=1), reads=["mk_ge"], writes=["mk_ge"])
        S.op("pool", lambda: nc.gpsimd.memset(g.negm[:], 0.0), writes=["negm"])
        S.op("pool", lambda: nc.gpsimd.affine_select(g.negm[:], g.negm[:], pattern=[[-1, 128]], compare_op=ALU.is_ge,
                                                      fill=NEG, base=0, channel_multiplier=1), reads=["negm"], writes=["negm"])
        S.dma("sp", g.permT[:], I["perm"].rearrange("c k m -> k c m"), writes=["permT"])
        S.dma("sp", g.fnorm[:], I["final_norm"][:, :], writes=["fnorm"])
        posi = g.sbt(st, "posi", [128, T], I32)
        posf = g.sbt(st, "posf", [128, T])
        fr = g.sbt(st, "fr", [128, 3])
        th = g.sbt(st, "th", [128, T])
        ki = g.sbt(st, "ki", [128, T], I32)
        kf = g.sbt(st, "kf", [128, T])
        rr = g.sbt(st, "rr", [128, T])
        S.dma("sp", posi[:], I["pos"][0:1, :].to_broadcast([128, T]), writes=["posi"])
        S.dma("sp", fr[:], I["freqs"][:, :], writes=["fr"])
        S.op("dve", lambda: nc.vector.tensor_copy(posf[:], posi[:]), reads=["posi"], writes=["posf"])
        TWO_PI = float(2 * np.pi)
        for cfg in range(3):
            for which in range(2):
                shift = float(np.pi / 2) if which == 0 else 0.0
                S.op("dve", lambda: nc.vector.tensor_scalar(th[:], posf[:], fr[:, cfg:cfg + 1], shift, op0=ALU.mult, op1=ALU.add),
                     reads=["posf", "fr"], writes=["th"])
                S.op("dve", lambda: nc.vector.tensor_scalar(ki[:], th[:], 1.0 / TWO_PI, None, op0=ALU.mult), reads=["th"], writes=["ki"])
                S.op("dve", lambda: nc.vector.tensor_copy(kf[:], ki[:]), reads=["ki"], writes=["kf"])
                S.op("dve", lambda: nc.vector.scalar_tensor_tensor(rr[:], kf[:], -TWO_PI, th[:], op0=ALU.mult, op1=ALU.add),
                     reads=["kf", "th"], writes=["rr"])
                S.op("dve", lambda: nc.vector.tensor_scalar(rr[:], rr[:], float(np.pi), float(-np.pi), op0=ALU.min, op1=ALU.max),
                     reads=["rr"], writes=["rr"])
                S.op("act", lambda: nc.scalar.activation(th[:], rr[:], AF.Sin), reads=["rr"], writes=["th"])
                r0 = (cfg * 2 + which) * 128
                S.dma("sp", R["CS"][r0:r0 + 128, :], th[:], reads=["th"], writes=["CS"], nowaw=True)
        cc = g.sbt(st, "cc", [128, KC])
        sc = g.sbt(st, "sc", [128, KC])
        S.dma("sp", cc[:], I["cc"][:, :], writes=["cc"])
        S.op("act", lambda: nc.scalar.activation(sc[:], cc[:], AF.Silu), reads=["cc"], writes=["sc"])
        modps = g.pst(st, "modps", [128, 6 * KC])
        NB = 256
        wa = [g.sbt(st, "wa%d" % i, [128, KC, NB]) for i in range(2)]
        wv = I["w_ada"].rearrange("(kc p) n -> p kc n", p=128)
        for cb in range(6 * D // NB):
            buf = wa[cb % 2]
            key = "wa%d" % (cb % 2)
            S.dma("sp", buf[:, 0:16, :], wv[:, 0:16, cb * NB:(cb + 1) * NB], writes=[key])
            S.dma("sp", buf[:, 16:32, :], wv[:, 16:32, cb * NB:(cb + 1) * NB], writes=[key], nowaw=True)
            for j4 in range(NB // 128):
                j = cb * (NB // 128) + j4
                for kc in range(KC):
                    S.op("pe", lambda: nc.tensor.matmul(modps[:, j:j + 1], buf[:, kc, j4 * 128:(j4 + 1) * 128], sc[:, kc:kc + 1],
                                                       start=(kc == 0), stop=(kc == KC - 1)),
                         reads=[key, "sc"], writes=["modps"], sig=(kc == KC - 1))
        badd = g.sbt(st, "badd", [128, 6 * KC])
        S.dma("sp", badd[:], I["b_ada"][:, :], writes=["badd"])
        for l in layers:
            L = str(l)
            tab = g.sbt(st, "tab" + L, [128, 6 * KC])
            nrm = g.sbt(st, "nrm" + L, [128, 2 * KC])
            S.dma("sp", tab[:], I["ada_table_" + L][:, :], writes=["tab" + L])
            S.dma("sp", nrm[:, 0:KC], I["mix_norm_" + L][:, :], writes=["nrm" + L])
            S.dma("sp", nrm[:, KC:2 * KC], I["ffn_norm_" + L][:, :], reads=[], writes=["nrmb" + L])
            mk = "mod%d" % l
            S.op("dve", lambda: nc.vector.tensor_tensor(g.mod[l][:], modps[:, 0:6 * KC], badd[:], op=ALU.add), reads=["modps", "badd"], writes=[mk])
            S.op("dve", lambda: nc.vector.tensor_tensor(g.mod[l][:], g.mod[l][:], tab[:], op=ALU.add), reads=[mk, "tab" + L], writes=[mk])
            ak = "AB%d" % l
            S.op("dve", lambda: nc.vector.scalar_tensor_tensor(g.AB[l][:, 0:KC], g.mod[l][:, KC:2 * KC], 1.0, nrm[:, 0:KC], op0=ALU.add, op1=ALU.mult),
                 reads=[mk, "nrm" + L], writes=[ak])
            S.op("dve", lambda: nc.vector.scalar_tensor_tensor(g.AB[l][:, KC:2 * KC], g.mod[l][:, 4 * KC:5 * KC], 1.0, nrm[:, KC:2 * KC], op0=ALU.add, op1=ALU.mult),
                 reads=[mk, "nrmb" + L], writes=[ak])
        S.barrier()


def norm_block(g, st, src, t0, ntok, A_ap, B_ap, dst, dst_key, nkc=KC, tile=128, tag="nb", extra=None, out_dram=None):
    nc, S = g.nc, g.S
    srcv = src.rearrange("(kc p) t -> p kc t", p=128)
    xb = [g.sbt(st, "%s_x%d" % (tag, i), [128, nkc, tile]) for i in range(2)]
    sq = g.sbt(st, tag + "_sq", [128, nkc, tile])
    ssq = g.pst(st, tag + "_ssq", [128, tile])
    rstd = g.sbt(st, tag + "_rstd", [128, tile])
    hf = g.sbt(st, tag + "_hf", [128, nkc, tile]) if (extra is not None or out_dram is not None) else None
    nfeat = float(nkc * 128)
    for ti in range(ntok // tile):
        x = xb[ti % 2]
        xk = "%s_x%d" % (tag, ti % 2)
        S.dma("sp", x[:], srcv[:, :, t0 + ti * tile:t0 + (ti + 1) * tile], writes=[xk])
        S.op("act", lambda: nc.scalar.activation(sq[:], x[:], AF.Square), reads=[xk], writes=[tag + "_sq"])
        for kc in range(nkc):
            S.op("pe", lambda: nc.tensor.matmul(ssq[:, 0:tile], g.onesf[:], sq[:, kc, :], start=(kc == 0), stop=(kc == nkc - 1)),
                 reads=[tag + "_sq", "onesf"], writes=[tag + "_ssq"], sig=(kc == nkc - 1))
        S.op("act", lambda: nc.scalar.activation(rstd[:], ssq[:, 0:tile], AF.Sqrt, bias=g.epsc[:, 0:1], scale=1.0 / nfeat),
             reads=[tag + "_ssq", "epsc"], writes=[tag + "_rstd"])
        S.op("dve", lambda: nc.vector.reciprocal(rstd[:], rstd[:]), reads=[tag + "_rstd"], writes=[tag + "_rstd"])
        S.op("dve", lambda: nc.vector.tensor_tensor(sq[:], x[:], rstd[:, None, :].to_broadcast([128, nkc, tile]), op=ALU.mult),
             reads=[xk, tag + "_rstd"], writes=[tag + "_sq"])
        for kc in range(nkc):
            bias = B_ap[:, kc:kc + 1] if B_ap is not None else 0.0
            if dst is not None:
                S.op("act", lambda: nc.scalar.activation(dst[:, kc, ti * tile:(ti + 1) * tile], sq[:, kc, :], AF.Identity,
                                                         bias=bias, scale=A_ap[:, kc:kc + 1]),
                     reads=[tag + "_sq"], writes=[dst_key], sig=(kc == nkc - 1))
            if hf is not None:
                S.op("act", lambda: nc.scalar.activation(hf[:, kc, :], sq[:, kc, :], AF.Identity, bias=bias, scale=A_ap[:, kc:kc + 1]),
                     reads=[tag + "_sq"], writes=[tag + "_hf"], sig=(kc == nkc - 1))
        if extra is not None:
            extra(ti, hf, tag + "_hf")
        if out_dram is not None:
            S.dma("sp", out_dram.rearrange("(kc p) t -> p kc t", p=128)[:, :, t0 + ti * tile:t0 + (ti + 1) * tile], hf[:],
                  reads=[tag + "_hf"], writes=["out_" + tag], nowaw=True)


def proj_fm(g, st, chunks, act, act_key, nkc, ntok, epi, tag="pf", wq="pool", nbank=4):
    nc, S = g.nc, g.S
    GRP = 2
    wb = [g.sbt(st, "%s_w%d" % (tag, i), [128, nkc, GRP * 128], BF16) for i in range(2)]
    ps = [g.pst(st, "%s_ps%d" % (tag, i), [128, 512]) for i in range(nbank)]
    pcnt = 0
    for gi in range(0, len(chunks), GRP):
        grp = chunks[gi:gi + GRP]
        bi = (gi // GRP) % 2
        wkey = "%s_w%d" % (tag, bi)
        first = True
        for j, (w_ap, n, info) in enumerate(grp):
            S.dma(wq, wb[bi][:, :, j * 128:j * 128 + n], w_ap.rearrange("(kc p) n -> p kc n", p=128), writes=[wkey], nowaw=not first)
            first = False
        for j, (w_ap, n, info) in enumerate(grp):
            for tt in range(ntok // 512):
                p = ps[pcnt % nbank]
                pkey = "%s_ps%d" % (tag, pcnt % nbank)
                pcnt += 1
                for kc in range(nkc):
                    S.op("pe", lambda: nc.tensor.matmul(p[0:n, :], wb[bi][:, kc, j * 128:j * 128 + n], act[:, kc, tt * 512:(tt + 1) * 512],
                                                       start=(kc == 0), stop=(kc == nkc - 1)),
                         reads=[wkey, act_key], writes=[pkey], sig=(kc == nkc - 1))
                epi(info, gi + j, tt, p[0:n, :], pkey)


def proj_tm(g, st, wblocks, act, act_key, nkc, ntok, epi, tag="pt", wq="pool", nbank=2, wmax=512):
    nc, S = g.nc, g.S
    wb = [g.sbt(st, "%s_w%d" % (tag, i), [128, nkc, wmax], BF16) for i in range(2)]
    ps = [g.pst(st, "%s_ps%d" % (tag, i), [128, 512]) for i in range(nbank)]
    pcnt = 0
    for bi_, (w_ap, n, info) in enumerate(wblocks):
        bi = bi_ % 2
        wkey = "%s_w%d" % (tag, bi)
        S.dma(wq, wb[bi][:, :, 0:n], w_ap.rearrange("(kc p) n -> p kc n", p=128), writes=[wkey])
        for ti in range(ntok // 128):
            p = ps[pcnt % nbank]
            pkey = "%s_ps%d" % (tag, pcnt % nbank)
            pcnt += 1
            for kc in range(nkc):
                S.op("pe", lambda: nc.tensor.matmul(p[:, 0:n], act[:, kc, ti * 128:(ti + 1) * 128], wb[bi][:, kc, 0:n],
                                                   start=(kc == 0), stop=(kc == nkc - 1)),
                     reads=[wkey, act_key], writes=[pkey], sig=(kc == nkc - 1))
            epi(info, bi_, ti, p[:, 0:n], pkey)


class Evac:
    def __init__(self, g, st, tag, shape, dt, n=4):
        self.g = g
        self.bufs = [g.sbt(st, "%s_e%d" % (tag, i), shape, dt) for i in range(n)]
        self.keys = ["%s_e%d" % (tag, i) for i in range(n)]
        self.i = 0

    def next(self):
        b, k = self.bufs[self.i % len(self.bufs)], self.keys[self.i % len(self.bufs)]
        self.i += 1
        return b, k


def ph_mixer_in(g, l, xsrc):
    nc, S, I, R = g.nc, g.S, g.I, g.R
    L = str(l)
    W = I["w_in_" + L]
    TB = 2048
    fm = []

    def add_fm(w, c0, ncols, dst, drow0, kind):
        for c in range(0, ncols, 128):
            n = min(128, ncols - c)
            fm.append((w[:, c0 + c:c0 + c + n], n, (dst, drow0 + c, kind)))

    for nm in ("a_q", "a_k", "b_cq", "b_ckv", "c_q", "c_k", "d_q", "d_k", "d_iq"):
        add_fm(W, OFF[nm][0], OFF[nm][1], "YF", YF_ROWS[nm], "copy")
    add_fm(I["w_kr2_" + L], 0, 128, "YF", YF_ROWS["kr2"], "copy")
    add_fm(I["w_ik2_" + L], 0, 128, "YF", YF_ROWS["ik2"], "copy")
    add_fm(W, OFF["g"][0], OFF["g"][1], "G", 0, "sig")
    tm = []
    for nm, dst in (("a_v", "VA"), ("c_v", "VC"), ("d_v", "VD"), ("d_iw", "IW")):
        c0, ncols = OFF[nm]
        for c in range(0, ncols, 256):
            n = min(256, ncols - c)
            tm.append((W[:, c0 + c:c0 + c + n], n, (dst, c)))
    for tb in range(T // TB):
        with ExitStack() as st:
            hT = g.sbt(st, "hT", [128, KC, TB], BF16)
            with ExitStack() as st2:
                norm_block(g, st2, xsrc, tb * TB, TB, g.AB[l][:, 0:KC], g.mod[l][:, 0:KC], hT, "hT")
                S.barrier()
            with ExitStack() as st2:
                ev = Evac(g, st2, "ev", [128, 512], F32, 4)
                evb = Evac(g, st2, "evb", [128, 512], BF16, 2)

                def epi_fm(info, ci, tt, p, pkey):
                    dst, row, kind = info
                    n = p.shape[0]
                    b, k = ev.next()
                    func = AF.Sigmoid if kind == "sig" else AF.Copy
                    S.op("act", lambda: nc.scalar.activation(b[0:n, :], p, func), reads=[pkey], writes=[k])
                    S.dma("sp", R[dst][row:row + n, tb * TB + tt * 512: tb * TB + (tt + 1) * 512], b[0:n, :], reads=[k], writes=[dst], nowaw=True)

                def epi_tm(info, bi, ti, p, pkey):
                    dst, c = info
                    n = p.shape[1]
                    t0 = tb * TB + ti * 128
                    if dst == "IW":
                        b, k = ev.next()
                    else:
                        b, k = evb.next()
                    S.op("dve", lambda: nc.vector.tensor_copy(b[:, 0:n], p), reads=[pkey], writes=[k])
                    S.dma("sp", R[dst][t0:t0 + 128, c:c + n], b[:, 0:n], reads=[k], writes=[dst], nowaw=True)

                with ExitStack() as st3:
                    proj_tm(g, st3, tm, hT, "hT", KC, TB, epi_tm, wmax=256)
                    S.barrier()
                with ExitStack() as st3:
                    proj_fm(g, st3, fm, hT, "hT", KC, TB, epi_fm)
                    S.barrier()


def host_consts():
    freqs = np.zeros((128, 3), np.float32)
    perm = np.zeros((3, 128, 128), np.float32)
    th = np.float32(THETA)

    def fr(half, rot):
        return (th ** (-(np.arange(half, dtype=np.float32) * np.float32(2.0 / rot)))).astype(np.float32)
    f = fr(16, 32)
    for p in range(32):
        freqs[p, 0] = f[p % 16]
    for m in range(16):
        perm[0][m + 16, m] = -1.0
        perm[0][m, m + 16] = 1.0
    f = fr(8, 16)
    for base in (0, 64):
        for p in range(16):
            freqs[base + p, 1] = f[p % 8]
        for m in range(8):
            perm[1][base + m + 8, base + m] = -1.0
            perm[1][base + m, base + m + 8] = 1.0
    f = fr(32, 64)
    for base in (0, 64):
        for p in range(64):
            freqs[base + p, 2] = f[p % 32]
        for m in range(32):
            perm[2][base + m + 32, base + m] = -1.0
            perm[2][base + m, base + m + 32] = 1.0
    return freqs, perm


def colvec(v, n=None):
    v = np.asarray(v, np.float32)
    return np.ascontiguousarray(v.reshape(-1, 128).T)


def prep_shared(inputs, layers=(0, 1)):
    m = {}
    freqs, perm = host_consts()
    m["freqs"], m["perm"] = freqs, perm
    m["w_ada"] = np.asarray(inputs["w_ada"], np.float32)
    m["b_ada"] = colvec(inputs["b_ada"])
    m["final_norm"] = colvec(inputs["final_norm"])
    qperm = np.array([h * 192 + j for h in range(8) for j in range(128)] + [h * 192 + 128 + j for h in range(8) for j in range(64)])
    kvperm = np.array([h * 256 + j for h in range(8) for j in range(128)] + [h * 256 + 128 + j for h in range(8) for j in range(128)])
    for l in layers:
        L = str(l)
        m["ada_table_" + L] = np.ascontiguousarray(np.asarray(inputs["ada_table_" + L], np.float32).reshape(6 * KC, 128).T)
        m["mix_norm_" + L] = colvec(inputs["mix_norm_" + L])
        w_in = np.asarray(inputs["w_in_" + L], np.float32)
        m["w_in_" + L] = w_in
        kr = w_in[:, OFF["b_kr"][0]:OFF["b_kr"][0] + 64]
        ik = w_in[:, OFF["d_ik"][0]:OFF["d_ik"][0] + 64]
        m["w_kr2_" + L] = np.ascontiguousarray(np.concatenate([kr, kr], axis=1))
        m["w_ik2_" + L] = np.ascontiguousarray(np.concatenate([ik, ik], axis=1))
        m["mla_q_norm_" + L] = colvec(inputs["mla_q_norm_" + L])
        m["mla_q_up_" + L] = np.ascontiguousarray(np.asarray(inputs["mla_q_up_" + L], np.float32)[:, qperm])
        m["mla_kv_norm_" + L] = colvec(inputs["mla_kv_norm_" + L])
        m["mla_kv_up_" + L] = np.ascontiguousarray(np.asarray(inputs["mla_kv_up_" + L], np.float32)[:, kvperm])
        m["w_branch_" + L] = np.asarray(inputs["w_branch_" + L], np.float32)
        m["w_out_" + L] = np.asarray(inputs["w_out_" + L], np.float32)
        m["ffn_norm_" + L] = colvec(inputs["ffn_norm_" + L])
        if l == 0:
            for k in ("ffn_gate_0", "ffn_up_0", "ffn_down_0"):
                m[k] = np.asarray(inputs[k], np.float32)
        else:
            m["router_1"] = np.asarray(inputs["router_1"], np.float32)
            m["expert_gate_1"] = np.asarray(inputs["expert_gate_1"], np.float32)
            m["expert_up_1"] = np.asarray(inputs["expert_up_1"], np.float32)
            m["expert_down_1"] = np.asarray(inputs["expert_down_1"], np.float32).reshape(N_EXP * D_FFE, D)
    return m


def prep_core(inputs, b):
    m = {}
    m["xT"] = np.ascontiguousarray(np.asarray(inputs["x"][b], np.float32).T)
    m["cc"] = colvec(inputs["c"][b])
    m["pos"] = np.ascontiguousarray(np.asarray(inputs["positions"][b], np.int32)[None, :])
    return m


class Rope:
    def __init__(self, g, st, cfg, tag):
        nc, S, R = g.nc, g.S, g.R
        self.g, self.cfg, self.tag = g, cfg, tag
        self.C = g.sbt(st, tag + "_C", [128, T])
        self.Sn = g.sbt(st, tag + "_S", [128, T])
        S.dma("sp", self.C[:], R["CS"][(cfg * 2) * 128:(cfg * 2 + 1) * 128, :], reads=["CS"], writes=[tag + "_C"])
        S.dma("sp", self.Sn[:], R["CS"][(cfg * 2 + 1) * 128:(cfg * 2 + 2) * 128, :], reads=["CS"], writes=[tag + "_S"])
        self.t1 = [g.sbt(st, "%s_t1%d" % (tag, i), [128, 512]) for i in range(2)]
        self.t2 = [g.sbt(st, "%s_t2%d" % (tag, i), [128, 512]) for i in range(2)]
        self.ps = [g.pst(st, "%s_ps%d" % (tag, i), [128, 512]) for i in range(2)]
        self.i = 0

    def apply(self, x_ap, x_key, t0, out_ap, out_key):
        g = self.g
        nc, S = g.nc, g.S
        i = self.i % 2
        self.i += 1
        tag = self.tag
        ps, t1, t2 = self.ps[i], self.t1[i], self.t2[i]
        pk, k1, k2 = "%s_ps%d" % (tag, i), "%s_t1%d" % (tag, i), "%s_t2%d" % (tag, i)
        S.op("pe", lambda: nc.tensor.matmul(ps[:], g.permT[:, self.cfg, :], x_ap, start=True, stop=True), reads=[x_key, "permT"], writes=[pk])
        S.op("dve", lambda: nc.vector.tensor_tensor(t1[:], x_ap, self.C[:, t0:t0 + 512], op=ALU.mult), reads=[x_key, tag + "_C"], writes=[k1])
        S.op("dve", lambda: nc.vector.tensor_tensor(t2[:], ps[:], self.Sn[:, t0:t0 + 512], op=ALU.mult), reads=[pk, tag + "_S"], writes=[k2])
        S.op("pool", lambda: nc.gpsimd.tensor_tensor(out_ap, t1[:], t2[:], op=ALU.add), reads=[k1, k2], writes=[out_key])


def rope_rows(g, st, rope, src_rows, dst_dram, dst_key, tag, dst_dt=BF16):
    nc, S = g.nc, g.S
    x = g.sbt(st, tag + "_x", [128, T])
    o = g.sbt(st, tag + "_o", [128, T], dst_dt)
    S.dma("sp", x[:], src_rows, reads=["YF"], writes=[tag + "_x"])
    for tt in range(T // 512):
        rope.apply(x[:, tt * 512:(tt + 1) * 512], tag + "_x", tt * 512, o[:, tt * 512:(tt + 1) * 512], tag + "_o")
    S.dma("sp", dst_dram, o[:], reads=[tag + "_o"], writes=[dst_key], nowaw=True)


def ph_mla(g, l):
    nc, S, I, R = g.nc, g.S, g.I, g.R
    L = str(l)
    with ExitStack() as st:
        qn = g.sbt(st, "qnrm", [128, 12])
        S.dma("sp", qn[:], I["mla_q_norm_" + L][:, :], writes=["qnrm"])
        cqn = g.sbt(st, "cqn", [128, 12, T], BF16)
        with ExitStack() as st2:
            norm_block(g, st2, R["YF"][YF_ROWS["b_cq"]:YF_ROWS["b_cq"] + 1536, :], 0, T, qn, None, cqn, "cqn", nkc=12, tile=256, tag="nq")
            S.barrier()
        with ExitStack() as st2:
            rope = Rope(g, st2, 2, "rq")
            ev = Evac(g, st2, "evq", [128, 512], BF16, 3)
            evf = Evac(g, st2, "evqf", [128, 512], F32, 2)
            Wq = I["mla_q_up_" + L]
            chunks = [(Wq[:, c * 128:(c + 1) * 128], 128, c) for c in range(12)]

            def epi(c, ci, tt, p, pkey):
                b, k = ev.next()
                if c < 8:
                    S.op("act", lambda: nc.scalar.copy(b[:], p), reads=[pkey], writes=[k])
                else:
                    xf, xk = evf.next()
                    S.op("act", lambda: nc.scalar.copy(xf[:], p), reads=[pkey], writes=[xk])
                    rope.apply(xf[:], xk, tt * 512, b[:], k)
                S.dma("sp", R["QB"][c * 128:(c + 1) * 128, tt * 512:(tt + 1) * 512], b[:], reads=[k], writes=["QB"], nowaw=True)

            proj_fm(g, st2, chunks, cqn, "cqn", 12, T, epi, tag="pq")
            S.barrier()
    with ExitStack() as st:
        kn = g.sbt(st, "kvnrm", [128, 4])
        S.dma("sp", kn[:], I["mla_kv_norm_" + L][:, :], writes=["kvnrm"])
        ckvn = g.sbt(st, "ckvn", [128, 4, T], BF16)
        with ExitStack() as st2:
            norm_block(g, st2, R["YF"][YF_ROWS["b_ckv"]:YF_ROWS["b_ckv"] + 512, :], 0, T, kn, None, ckvn, "ckvn", nkc=4, tile=512, tag="nk")
            S.barrier()
        with ExitStack() as st2:
            ev = Evac(g, st2, "evk", [128, 512], BF16, 4)
            Wkv = I["mla_kv_up_" + L]
            chunks = [(Wkv[:, c * 128:(c + 1) * 128], 128, c) for c in range(8)]

            def epi(c, ci, tt, p, pkey):
                b, k = ev.next()
                S.op("act", lambda: nc.scalar.copy(b[:], p), reads=[pkey], writes=[k])
                S.dma("sp", R["KB"][c * 128:(c + 1) * 128, tt * 512:(tt + 1) * 512], b[:], reads=[k], writes=["KB"], nowaw=True)

            proj_fm(g, st2, chunks, ckvn, "ckvn", 4, T, epi, tag="pk")
            blocks = [(Wkv[:, 1024 + c * 512:1024 + (c + 1) * 512], 512, c) for c in range(2)]

            def epi_v(c, bi, ti, p, pkey):
                b, k = ev.next()
                S.op("dve", lambda: nc.vector.tensor_copy(b[:], p), reads=[pkey], writes=[k])
                S.dma("sp", R["VB"][ti * 128:(ti + 1) * 128, c * 512:(c + 1) * 512], b[:], reads=[k], writes=["VB"], nowaw=True)

            proj_tm(g, st2, blocks, ckvn, "ckvn", 4, T, epi_v, tag="pv")
            S.barrier()
    with ExitStack() as st:
        rope = Rope(g, st, 2, "rk")
        rope_rows(g, st, rope, R["YF"][YF_ROWS["kr2"]:YF_ROWS["kr2"] + 128, :], R["KPE"][:, :], "KPE", "rkr")
        S.barrier()
    scale = float((128 + 64) ** -0.5)
    with ExitStack() as st:
        kp = g.sbt(st, "kp", [128, T], BF16)
        S.dma("sp", kp[:], R["KPE"][:, :], reads=["KPE"], writes=["kp"])
        bufs = [{n: g.sbt(st, "%s%d" % (n, i), [128, T], BF16) for n in ("qn", "qp", "kn")} for i in range(2)]
        vb = [g.sbt(st, "vh%d" % i, [128, NT, 128], BF16) for i in range(2)]
        sps = [g.pst(st, "sps%d" % i, [128, 512]) for i in range(2)]
        ops_ = g.pst(st, "ops", [128, 512])
        dps = g.pst(st, "dps", [128, 512])
        pts = [g.sbt(st, "pt%d" % i, [128, 512], BF16) for i in range(3)]
        rd = g.sbt(st, "rd", [128, 512])
        ob = [g.sbt(st, "ob%d" % i, [128, 512], BF16) for i in range(2)]
        it = 0
        oi = 0
        for h in range(8):
            bi = h % 2
            B = bufs[bi]
            S.dma("sp", B["qn"][:], R["QB"][h * 128:(h + 1) * 128, :], reads=["QB"], writes=["qn%d" % bi])
            S.dma("sp", B["qp"][:], R["QB"][1024 + (h // 2) * 128:1024 + (h // 2 + 1) * 128, :], reads=["QB"], writes=["qp%d" % bi])
            S.dma("sp", B["kn"][:], R["KB"][h * 128:(h + 1) * 128, :], reads=["KB"], writes=["kn%d" % bi])
            S.dma("sp", vb[bi][:], R["VB"][:, h * 128:(h + 1) * 128].rearrange("(n p) c -> p n c", p=128), reads=["VB"], writes=["vh%d" % bi])
            pb = 64 * (h % 2)
            for qb in range(T // 512):
                q0 = qb * 512
                nkt = 4 * qb + 4
                for kt in range(nkt):
                    i = kt - 4 * qb
                    c0 = max(i, 0) * 128
                    sp_ = sps[it % 2]
                    sk = "sps%d" % (it % 2)
                    pt = pts[it % 3]
                    pk = "pt%d" % (it % 3)
                    it += 1
                    S.op("pe", lambda: nc.tensor.matmul(sp_[:, c0:512], B["kn"][:, kt * 128:(kt + 1) * 128], B["qn"][:, q0 + c0:q0 + 512], start=True, stop=False),
                         reads=["kn%d" % bi, "qn%d" % bi], writes=[sk], sig=False)
                    S.op("pe", lambda: nc.tensor.matmul(sp_[:, c0:512], kp[pb:pb + 64, kt * 128:(kt + 1) * 128], B["qp"][pb:pb + 64, q0 + c0:q0 + 512], start=False, stop=True),
                         reads=["kp", "qp%d" % bi], writes=[sk])
                    S.op("act", lambda: nc.scalar.activation(pt[:, c0:512], sp_[:, c0:512], AF.Exp, scale=scale), reads=[sk], writes=[pk])
                    if i >= 0:
                        S.op("dve", lambda: nc.vector.tensor_tensor(pt[:, c0:c0 + 128], pt[:, c0:c0 + 128], g.mk_le[:], op=ALU.mult), reads=[pk, "mk_le"], writes=[pk])
                    S.op("pe", lambda: nc.tensor.matmul(ops_[:, c0:512], vb[bi][:, kt, :], pt[:, c0:512], start=(kt == 0), stop=(kt == nkt - 1)),
                         reads=["vh%d" % bi, pk], writes=["ops"], sig=False)
                    S.op("pe", lambda: nc.tensor.matmul(dps[:, c0:512], g.ones[:], pt[:, c0:512], start=(kt == 0), stop=(kt == nkt - 1)),
                         reads=["ones", pk], writes=["dps"])
                o = ob[oi % 2]
                ok = "ob%d" % (oi % 2)
                oi += 1
                S.op("dve", lambda: nc.vector.reciprocal(rd[:], dps[:]), reads=["dps"], writes=["rd"])
                S.op("dve", lambda: nc.vector.tensor_tensor(o[:], ops_[:], rd[:], op=ALU.mult), reads=["ops", "rd"], writes=[ok])
                S.dma("sp", R["OT"][512 + h * 128:512 + (h + 1) * 128, q0:q0 + 512], o[:], reads=[ok], writes=["OT"], nowaw=True)
        S.barrier()


def build_maskT(g, Mq, mq_key, nkt, MT, mt_key, trp, trp_key):
    nc, S = g.nc, g.S
    for k0 in range(0, nkt, 4):
        n = min(4, nkt - k0)
        for j in range(n):
            S.op("pe", lambda: nc.tensor.transpose(trp[:, j * 128:(j + 1) * 128], Mq[:, (k0 + j) * 128:(k0 + j + 1) * 128], g.ident[:]),
                 reads=[mq_key, "ident"], writes=[trp_key], sig=(j == n - 1))
        S.op("act", lambda: nc.scalar.copy(MT[:, k0:k0 + n, :], trp[:, 0:n * 128].rearrange("p (a b) -> p a b", b=128)), reads=[trp_key], writes=[mt_key])


def ph_moba(g, l):
    nc, S, I, R = g.nc, g.S, g.I, g.R
    scale = float(128 ** -0.5)
    with ExitStack() as st:
        rope = Rope(g, st, 0, "rc")
        xin = g.sbt(st, "mb_x", [128, T])
        qf = g.sbt(st, "mb_qf", [128, T])
        kf = g.sbt(st, "mb_kf", [128, T])
        qb = g.sbt(st, "mb_qb", [128, T], BF16)
        kb = g.sbt(st, "mb_kb", [128, T], BF16)
        vv = g.sbt(st, "mb_v", [128, NT, 128], BF16)
        kmean = g.sbt(st, "mb_km", [128, 16])
        gm = g.sbt(st, "mb_gm", [128, 16])
        m8 = g.sbt(st, "mb_m8", [128, 8])
        sel = g.sbt(st, "mb_sel", [128, 16])
        Mq = g.sbt(st, "mb_Mq", [128, T], BF16)
        MT = g.sbt(st, "mb_MT", [128, NT, 128], BF16)
        gps = g.pst(st, "mb_gps", [128, 16])
        trp = g.pst(st, "mb_trp", [128, 512], BF16)
        sps = [g.pst(st, "mb_sps%d" % i, [128, 512]) for i in range(2)]
        ops_ = g.pst(st, "mb_ops", [128, 128])
        dps = g.pst(st, "mb_dps", [128, 128])
        pts = [g.sbt(st, "mb_pt%d" % i, [128, 512], BF16) for i in range(2)]
        rd = g.sbt(st, "mb_rd", [128, 128])
        ob = [g.sbt(st, "mb_ob%d" % i, [128, 512], BF16) for i in range(2)]
        it = 0
        for h in range(8):
            for (nm, dstf, dstb, fk, bk) in (("c_q", qf, qb, "mb_qf", "mb_qb"), ("c_k", kf, kb, "mb_kf", "mb_kb")):
                r0 = YF_ROWS[nm] + h * 128
                S.dma("sp", xin[:], R["YF"][r0:r0 + 128, :], reads=["YF"], writes=["mb_x"])
                for tt in range(T // 512):
                    rope.apply(xin[:, tt * 512:(tt + 1) * 512], "mb_x", tt * 512, dstf[:, tt * 512:(tt + 1) * 512], fk)
                S.op("act", lambda: nc.scalar.copy(dstb[:], dstf[:]), reads=[fk], writes=[bk])
            S.dma("sp", vv[:], R["VC"][:, h * 128:(h + 1) * 128].rearrange("(n p) c -> p n c", p=128), reads=["VC"], writes=["mb_v"])
            S.op("dve", lambda: nc.vector.tensor_reduce(out=kmean[:], in_=kf[:].rearrange("p (a b) -> p a b", b=256), axis=AX.X, op=ALU.add),
                 reads=["mb_kf"], writes=["mb_km"])
            S.op("dve", lambda: nc.vector.tensor_scalar(kmean[:], kmean[:], 1.0 / 256.0, None, op0=ALU.mult), reads=["mb_km"], writes=["mb_km"])
            S.op("dve", lambda: nc.vector.memset(gm[:], NEG), writes=["mb_gm"])
            for i in range(NT):
                own = i // 2
                nkt = i + 1
                if own > 3:
                    S.op("pe", lambda: nc.tensor.matmul(gps[:, 0:16], qf[:, i * 128:(i + 1) * 128], kmean[:], start=True, stop=True),
                         reads=["mb_qf", "mb_km"], writes=["mb_gps"])
                    S.op("dve", lambda: nc.vector.tensor_copy(gm[:, 0:own], gps[:, 0:own]), reads=["mb_gps"], writes=["mb_gm"])
                    S.op("dve", lambda: nc.vector.max(out=m8[:], in_=gm[:]), reads=["mb_gm"], writes=["mb_m8"])
                    S.op("dve", lambda: nc.vector.tensor_scalar(sel[:], gm[:], m8[:, 2:3], None, op0=ALU.is_ge), reads=["mb_gm", "mb_m8"], writes=["mb_sel"])
                    S.op("dve", lambda: nc.vector.tensor_copy(Mq[:, 0:own * 256].rearrange("p (a b) -> p a b", b=256),
                                                              sel[:, 0:own, None].to_broadcast([128, own, 256])), reads=["mb_sel"], writes=["mb_Mq"])
                elif own > 0:
                    S.op("dve", lambda: nc.vector.memset(Mq[:, 0:own * 256], 1.0), writes=["mb_Mq"])
                if i % 2 == 1:
                    S.op("dve", lambda: nc.vector.memset(Mq[:, own * 256:own * 256 + 128], 1.0), writes=["mb_Mq"])
                S.op("dve", lambda: nc.vector.tensor_copy(Mq[:, i * 128:(i + 1) * 128], g.mk_ge[:]), reads=["mk_ge"], writes=["mb_Mq"])
                build_maskT(g, Mq, "mb_Mq", nkt, MT, "mb_MT", trp, "mb_trp")
                for k0 in range(0, nkt, 4):
                    n = min(4, nkt - k0)
                    sp_ = sps[it % 2]
                    sk = "mb_sps%d" % (it % 2)
                    pt = pts[it % 2]
                    pk = "mb_pt%d" % (it % 2)
                    it += 1
                    for j in range(n):
                        S.op("pe", lambda: nc.tensor.matmul(sp_[:, j * 128:(j + 1) * 128], kb[:, (k0 + j) * 128:(k0 + j + 1) * 128], qb[:, i * 128:(i + 1) * 128],
                                                           start=True, stop=True), reads=["mb_kb", "mb_qb"], writes=[sk], sig=(j == n - 1))
                    S.op("act", lambda: nc.scalar.activation(pt[:, 0:n * 128], sp_[:, 0:n * 128], AF.Exp, scale=scale), reads=[sk], writes=[pk])
                    S.op("dve", lambda: nc.vector.tensor_tensor(pt[:, 0:n * 128], pt[:, 0:n * 128], MT[:, k0:k0 + n, :].rearrange("p a b -> p (a b)"), op=ALU.mult),
                         reads=[pk, "mb_MT"], writes=[pk])
                    for j in range(n):
                        kt = k0 + j
                        S.op("pe", lambda: nc.tensor.matmul(ops_[:, 0:128], vv[:, kt, :], pt[:, j * 128:(j + 1) * 128], start=(kt == 0), stop=(kt == nkt - 1)),
                             reads=["mb_v", pk], writes=["mb_ops"], sig=False)
                        S.op("pe", lambda: nc.tensor.matmul(dps[:, 0:128], g.ones[:], pt[:, j * 128:(j + 1) * 128], start=(kt == 0), stop=(kt == nkt - 1)),
                             reads=["ones", pk], writes=["mb_dps"], sig=(j == n - 1))
                o = ob[(i // 4) % 2]
                ok = "mb_ob%d" % ((i // 4) % 2)
                S.op("dve", lambda: nc.vector.reciprocal(rd[:], dps[:, 0:128]), reads=["mb_dps"], writes=["mb_rd"])
                S.op("dve", lambda: nc.vector.tensor_tensor(o[:, (i % 4) * 128:(i % 4 + 1) * 128], ops_[:, 0:128], rd[:], op=ALU.mult), reads=["mb_ops", "mb_rd"], writes=[ok])
                if i % 4 == 3:
                    q0 = (i // 4) * 512
                    S.dma("sp", R["OT"][1536 + h * 128:1536 + (h + 1) * 128, q0:q0 + 512], o[:], reads=[ok], writes=["OT"], nowaw=True)
        S.barrier()


def ph_dsa(g, l):
    nc, S, I, R = g.nc, g.S, g.I, g.R
    scale = float(128 ** -0.5)
    Qd = None
    with ExitStack() as st:
        Qd = g.sbt(st, "ds_Q", [128, 8, T], BF16)
        Kd = g.sbt(st, "ds_K", [128, T], BF16)
        ikf = g.sbt(st, "ds_ik", [128, T])
        Vd = g.sbt(st, "ds_V", [128, NT, 128], BF16)
        with ExitStack() as st2:
            rope0 = Rope(g, st2, 0, "rd0")
            xin = g.sbt(st2, "ds_x", [128, T])
            for h in range(9):
                r0 = YF_ROWS["d_q"] + h * 128 if h < 8 else YF_ROWS["d_k"]
                S.dma("sp", xin[:], R["YF"][r0:r0 + 128, :], reads=["YF"], writes=["ds_x"])
                for tt in range(T // 512):
                    dst = Qd[:, h, tt * 512:(tt + 1) * 512] if h < 8 else Kd[:, tt * 512:(tt + 1) * 512]
                    rope0.apply(xin[:, tt * 512:(tt + 1) * 512], "ds_x", tt * 512, dst, "ds_Q" if h < 8 else "ds_K")
            S.barrier()
        with ExitStack() as st2:
            rope1 = Rope(g, st2, 1, "rd1")
            xin = g.sbt(st2, "ds_x2", [128, T])
            xo = g.sbt(st2, "ds_xo", [128, T])
            for c in range(17):
                r0 = YF_ROWS["d_iq"] + c * 128 if c < 16 else YF_ROWS["ik2"]
                S.dma("sp", xin[:], R["YF"][r0:r0 + 128, :], reads=["YF"], writes=["ds_x2"])
                dstt, dk = (xo, "ds_xo") if c < 16 else (ikf, "ds_ik")
                for tt in range(T // 512):
                    rope1.apply(xin[:, tt * 512:(tt + 1) * 512], "ds_x2", tt * 512, dstt[:, tt * 512:(tt + 1) * 512], dk)
                if c < 16:
                    S.dma("sp", R["IQR"][c * 128:(c + 1) * 128, :], xo[:], reads=["ds_xo"], writes=["IQR"], nowaw=True)
            S.barrier()
        S.dma("sp", Vd[:], R["VD"][:, :].rearrange("(n p) c -> p n c", p=128), reads=["VD"], writes=["ds_V"])
        iqt = [g.sbt(st, "ds_iq%d" % i, [128, 16, 128]) for i in range(2)]
        iwt = g.sbt(st, "ds_iw", [128, 32])
        aw = g.sbt(st, "ds_aw", [128, 32])
        sg = g.sbt(st, "ds_sg", [128, 32])
        acc = g.sbt(st, "ds_acc", [128, T])
        wk = g.sbt(st, "ds_wk", [128, T])
        rl = [g.sbt(st, "ds_rl%d" % i, [128, 512]) for i in range(2)]
        m8 = g.sbt(st, "ds_m8", [128, 8])
        Mq = g.sbt(st, "ds_Mq", [128, T], BF16)
        MT = g.sbt(st, "ds_MT", [128, NT, 128], BF16)
        ips = [g.pst(st, "ds_ips%d" % i, [128, 512]) for i in range(2)]
        trp = g.pst(st, "ds_trp", [128, 512], BF16)
        sps = [g.pst(st, "ds_sps%d" % i, [128, 512]) for i in range(2)]
        ops_ = g.pst(st, "ds_ops", [128, 512])
        dps = g.pst(st, "ds_dps", [128, 512])
        pts = [g.sbt(st, "ds_pt%d" % i, [128, 512], BF16) for i in range(2)]
        rd = g.sbt(st, "ds_rd", [128, 512])
        ob = [g.sbt(st, "ds_ob%d" % i, [128, 4, 128], BF16) for i in range(2)]
        cidx = float((32 ** -0.5) * (64 ** -0.5))
        it = 0
        ii = 0
        oi = 0
        for i in range(NT):
            nkt = i + 1
            ns = nkt * 128
            if i >= 2:
                iq = iqt[i % 2]
                iqk = "ds_iq%d" % (i % 2)
                S.dma("sp", iq[:], R["IQR"][:, i * 128:(i + 1) * 128].rearrange("(c p) t -> p c t", p=128), reads=["IQR"], writes=[iqk])
                S.dma("sp", iwt[:], R["IW"][i * 128:(i + 1) * 128, :], reads=["IW"], writes=["ds_iw"])
                S.op("act", lambda: nc.scalar.activation(aw[:], iwt[:], AF.Abs, scale=cidx), reads=["ds_iw"], writes=["ds_aw"])
                S.op("act", lambda: nc.scalar.activation(sg[:], iwt[:], AF.Sign), reads=["ds_iw"], writes=["ds_sg"])
                for hh in range(32):
                    pb = 64 * (hh % 2)
                    for s0 in range(0, ns, 512):
                        n = min(512, ns - s0)
                        ip = ips[ii % 2]
                        ik_ = "ds_ips%d" % (ii % 2)
                        r = rl[ii % 2]
                        rk = "ds_rl%d" % (ii % 2)
                        ii += 1
                        S.op("pe", lambda: nc.tensor.matmul(ip[:, 0:n], iq[pb:pb + 64, hh // 2, :], ikf[pb:pb + 64, s0:s0 + n], start=True, stop=True),
                             reads=[iqk, "ds_ik"], writes=[ik_])
                        S.op("act", lambda: nc.scalar.activation(r[:, 0:n], ip[:, 0:n], AF.Relu, scale=aw[:, hh:hh + 1]), reads=[ik_, "ds_aw"], writes=[rk])
                        if hh == 0:
                            S.op("dve", lambda: nc.vector.tensor_scalar(acc[:, s0:s0 + n], r[:, 0:n], sg[:, 0:1], None, op0=ALU.mult),
                                 reads=[rk, "ds_sg"], writes=["ds_acc"])
                        else:
                            S.op("dve", lambda: nc.vector.scalar_tensor_tensor(acc[:, s0:s0 + n], r[:, 0:n], sg[:, hh:hh + 1], acc[:, s0:s0 + n], op0=ALU.mult, op1=ALU.add),
                                 reads=[rk, "ds_sg", "ds_acc"], writes=["ds_acc"])
                S.op("dve", lambda: nc.vector.tensor_tensor(acc[:, i * 128:ns], acc[:, i * 128:ns], g.negm[:], op=ALU.add), reads=["ds_acc", "negm"], writes=["ds_acc"])
                src = acc
                for rnd in range(32):
                    S.op("dve", lambda: nc.vector.max(out=m8[:], in_=src[:, 0:ns]), reads=["ds_acc", "ds_wk"], writes=["ds_m8"])
                    if rnd < 31:
                        S.op("dve", lambda: nc.vector.match_replace(out=wk[:, 0:ns], in_to_replace=m8[:], in_values=src[:, 0:ns], imm_value=NEG),
                             reads=["ds_acc", "ds_wk", "ds_m8"], writes=["ds_wk"])
                        src = wk
                S.op("dve", lambda: nc.vector.tensor_scalar(Mq[:, 0:ns], acc[:, 0:ns], m8[:, 7:8], None, op0=ALU.is_ge), reads=["ds_acc", "ds_m8"], writes=["ds_Mq"])
            else:
                if i == 1:
                    S.op("dve", lambda: nc.vector.memset(Mq[:, 0:128], 1.0), writes=["ds_Mq"])
                S.op("dve", lambda: nc.vector.tensor_copy(Mq[:, i * 128:(i + 1) * 128], g.mk_ge[:]), reads=["mk_ge"], writes=["ds_Mq"])
            build_maskT(g, Mq, "ds_Mq", nkt, MT, "ds_MT", trp, "ds_trp")
            for hg in range(2):
                for kt in range(nkt):
                    sp_ = sps[it % 2]
                    sk = "ds_sps%d" % (it % 2)
                    pt = pts[it % 2]
                    pk = "ds_pt%d" % (it % 2)
                    it += 1
                    S.op("pe", lambda: nc.tensor.matmul(sp_[:].rearrange("p (a b) -> p a b", b=128), Kd[:, kt * 128:(kt + 1) * 128], Qd[:, hg * 4:(hg + 1) * 4, i * 128:(i + 1) * 128],
                                                       start=True, stop=True), reads=["ds_K", "ds_Q"], writes=[sk])
                    S.op("act", lambda: nc.scalar.activation(pt[:], sp_[:], AF.Exp, scale=scale), reads=[sk], writes=[pk])
                    S.op("dve", lambda: nc.vector.tensor_tensor(pt[:].rearrange("p (a b) -> p a b", b=128), pt[:].rearrange("p (a b) -> p a b", b=128),
                                                                MT[:, kt:kt + 1, :].to_broadcast([128, 4, 128]), op=ALU.mult), reads=[pk, "ds_MT"], writes=[pk])
                    S.op("pe", lambda: nc.tensor.matmul(ops_[:], Vd[:, kt, :], pt[:], start=(kt == 0), stop=(kt == nkt - 1)), reads=["ds_V", pk], writes=["ds_ops"], sig=False)
                    S.op("pe", lambda: nc.tensor.matmul(dps[:], g.ones[:], pt[:], start=(kt == 0), stop=(kt == nkt - 1)), reads=["ones", pk], writes=["ds_dps"])
                o = ob[oi % 2]
                ok = "ds_ob%d" % (oi % 2)
                oi += 1
                S.op("dve", lambda: nc.vector.reciprocal(rd[:], dps[:]), reads=["ds_dps"], writes=["ds_rd"])
                S.op("dve", lambda: nc.vector.tensor_tensor(o[:].rearrange("p a b -> p (a b)"), ops_[:], rd[:], op=ALU.mult), reads=["ds_ops", "ds_rd"], writes=[ok])
                r0 = 2560 + hg * 512
                S.dma("sp", R["OT"][r0:r0 + 512, i * 128:(i + 1) * 128].rearrange("(a p) t -> p a t", p=128), o[:], reads=[ok], writes=["OT"], nowaw=True)
        S.barrier()


def ph_dil(g, l):
    nc, S, I, R = g.nc, g.S, g.I, g.R
    scale = float(128 ** -0.5)
    GROUPS = ((128, 1), (512, 4), (2048, 16))
    with ExitStack() as st:
        rope = Rope(g, st, 0, "ra")
        xin = g.sbt(st, "dl_x", [128, T])
        xr = g.sbt(st, "dl_xr", [128, T])
        qb = g.sbt(st, "dl_qb", [128, T], BF16)
        kb = g.sbt(st, "dl_kb", [128, T], BF16)
        Vc = g.sbt(st, "dl_V", [128, NT, 128], BF16)
        Oacc = g.sbt(st, "dl_O", [128, T])
        Dacc = g.sbt(st, "dl_D", [128, T])
        m2 = g.sbt(st, "dl_m2", [128, 256], BF16)
        sps = [g.pst(st, "dl_sps%d" % i, [128, 512]) for i in range(2)]
        ops_ = [g.pst(st, "dl_ops%d" % i, [128, 512]) for i in range(2)]
        dps = [g.pst(st, "dl_dps%d" % i, [128, 512]) for i in range(2)]
        pts = [g.sbt(st, "dl_pt%d" % i, [128, 256], BF16) for i in range(2)]
        ob = g.sbt(st, "dl_ob", [128, T], BF16)
        S.op("dve", lambda: nc.vector.tensor_copy(m2[:, 0:128], g.mk_le[:]), reads=["mk_le"], writes=["dl_m2"])
        S.op("dve", lambda: nc.vector.tensor_copy(m2[:, 128:256], g.mk_ge[:]), reads=["mk_ge"], writes=["dl_m2"])
        it = 0
        for u in range(4):
            for gi, (window, d) in enumerate(GROUPS):
                hidx = gi * 4 + u
                nb = T // (128 * d)
                for (nm, dstb, bk) in (("a_q", qb, "dl_qb"), ("a_k", kb, "dl_kb")):
                    r0 = YF_ROWS[nm] + hidx * 128
                    S.dma("sp", xin[:], R["YF"][r0:r0 + 128, :], reads=["YF"], writes=["dl_x"])
                    for tt in range(T // 512):
                        rope.apply(xin[:, tt * 512:(tt + 1) * 512], "dl_x", tt * 512, dstb[:, tt * 512:(tt + 1) * 512], bk)
                vsrc = R["VA"][:, hidx * 128:(hidx + 1) * 128].rearrange("(j i r) c -> i r j c", i=128, r=d)
                for r in range(d):
                    S.dma("sp", Vc[:, r * nb:(r + 1) * nb, :], vsrc[:, r, :, :], reads=["VA"], writes=["dl_V"], nowaw=(r > 0))
                for r in range(d):
                    for j in range(nb):
                        def cols(jj):
                            a0 = r + d * 128 * jj
                            return slice(a0, a0 + d * 127 + 1, d)
                        qs = cols(j)
                        n = 256 if j > 0 else 128
                        sp_ = sps[it % 2]
                        sk = "dl_sps%d" % (it % 2)
                        pt = pts[it % 2]
                        pk = "dl_pt%d" % (it % 2)
                        op_ = ops_[it % 2]
                        okk = "dl_ops%d" % (it % 2)
                        dp_ = dps[it % 2]
                        dk = "dl_dps%d" % (it % 2)
                        it += 1
                        S.op("pe", lambda: nc.tensor.matmul(sp_[:, 0:128], kb[:, qs], qb[:, qs], start=True, stop=True), reads=["dl_kb", "dl_qb"], writes=[sk], sig=(j == 0))
                        if j > 0:
                            S.op("pe", lambda: nc.tensor.matmul(sp_[:, 128:256], kb[:, cols(j - 1)], qb[:, qs], start=True, stop=True), reads=["dl_kb", "dl_qb"], writes=[sk])
                        S.op("act", lambda: nc.scalar.activation(pt[:, 0:n], sp_[:, 0:n], AF.Exp, scale=scale), reads=[sk], writes=[pk])
                        S.op("dve", lambda: nc.vector.tensor_tensor(pt[:, 0:n], pt[:, 0:n], m2[:, 0:n], op=ALU.mult), reads=[pk, "dl_m2"], writes=[pk])
                        S.op("pe", lambda: nc.tensor.matmul(op_[:, 0:128], Vc[:, r * nb + j, :], pt[:, 0:128], start=True, stop=(j == 0)), reads=["dl_V", pk], writes=[okk], sig=False)
                        if j > 0:
                            S.op("pe", lambda: nc.tensor.matmul(op_[:, 0:128], Vc[:, r * nb + j - 1, :], pt[:, 128:256], start=False, stop=True), reads=["dl_V", pk], writes=[okk], sig=False)
                        S.op("pe", lambda: nc.tensor.matmul(dp_[:, 0:128], g.ones[:], pt[:, 0:128], start=True, stop=(j == 0)), reads=["ones", pk], writes=[dk], sig=(j == 0))
                        if j > 0:
                            S.op("pe", lambda: nc.tensor.matmul(dp_[:, 0:128], g.ones[:], pt[:, 128:256], start=False, stop=True), reads=["ones", pk], writes=[dk])
                        if gi == 0:
                            S.op("dve", lambda: nc.vector.tensor_copy(Oacc[:, qs], op_[:, 0:128]), reads=[okk], writes=["dl_O"])
                            S.op("dve", lambda: nc.vector.tensor_copy(Dacc[:, qs], dp_[:, 0:128]), reads=[dk], writes=["dl_D"])
                        else:
                            S.op("dve", lambda: nc.vector.tensor_tensor(Oacc[:, qs], Oacc[:, qs], op_[:, 0:128], op=ALU.add), reads=[okk, "dl_O"], writes=["dl_O"])
                            S.op("dve", lambda: nc.vector.tensor_tensor(Dacc[:, qs], Dacc[:, qs], dp_[:, 0:128], op=ALU.add), reads=[dk, "dl_D"], writes=["dl_D"])
            S.op("dve", lambda: nc.vector.reciprocal(Dacc[:], Dacc[:]), reads=["dl_D"], writes=["dl_D"])
            S.op("dve", lambda: nc.vector.tensor_tensor(ob[:], Oacc[:], Dacc[:], op=ALU.mult), reads=["dl_O", "dl_D"], writes=["dl_ob"])
            S.dma("sp", R["OT"][u * 128:(u + 1) * 128, :], ob[:], reads=["dl_ob"], writes=["OT"], nowaw=True)
        S.barrier()


def ph_branch(g, l):
    nc, S, I, R = g.nc, g.S, g.I, g.R
    L = str(l)
    TB = 2048
    NK = 28
    br = ((0, 4), (4, 12), (12, 20), (20, 28))
    Wb = I["w_branch_" + L]
    Gv = R["G"].rearrange("(b f) t -> f b t", b=4)
    for tb in range(T // TB):
        with ExitStack() as st:
            oT = g.sbt(st, "br_oT", [128, NK, TB], BF16)
            for k0 in range(0, NK, 7):
                S.dma("sp", oT[:, k0:k0 + 7, :], R["OT"][k0 * 128:(k0 + 7) * 128, tb * TB:(tb + 1) * TB].rearrange("(kc p) t -> p kc t", p=128),
                      reads=["OT"], writes=["br_oT"], nowaw=(k0 > 0))
            wb = [g.sbt(st, "br_w%d" % i, [128, NK, 128], BF16) for i in range(2)]
            gt = [g.sbt(st, "br_g%d" % i, [128, 4, 512]) for i in range(2)]
            ps = [g.pst(st, "br_ps%d" % i, [128, 512]) for i in range(8)]
            tmp = [g.sbt(st, "br_t%d" % i, [128, 512]) for i in range(4)]
            yb = [g.sbt(st, "br_y%d" % i, [128, 512], BF16) for i in range(2)]
            it = 0
            for c in range(KC):
                w = wb[c % 2]
                wk = "br_w%d" % (c % 2)
                S.dma("pool", w[:], Wb[:, c * 128:(c + 1) * 128].rearrange("(kc p) n -> p kc n", p=128), writes=[wk])
                for tt in range(TB // 512):
                    t0 = tb * TB + tt * 512
                    gg = gt[it % 2]
                    gk = "br_g%d" % (it % 2)
                    y = yb[it % 2]
                    yk = "br_y%d" % (it % 2)
                    pb = (it % 2) * 4
                    it += 1
                    S.dma("sp", gg[:], Gv[c * 128:(c + 1) * 128, :, t0:t0 + 512], reads=["G"], writes=[gk])
                    for b, (k0, k1) in enumerate(br):
                        for kc in range(k0, k1):
                            S.op("pe", lambda: nc.tensor.matmul(ps[pb + b][:], w[:, kc, :], oT[:, kc, tt * 512:(tt + 1) * 512], start=(kc == k0), stop=(kc == k1 - 1)),
                                 reads=[wk, "br_oT"], writes=["br_ps%d" % (pb + b)], sig=(kc == k1 - 1))
                    for b in range(4):
                        S.op("dve", lambda: nc.vector.tensor_tensor(tmp[b][:], ps[pb + b][:], gg[:, b, :], op=ALU.mult), reads=["br_ps%d" % (pb + b), gk], writes=["br_t%d" % b])
                    S.op("pool", lambda: nc.gpsimd.tensor_tensor(tmp[0][:], tmp[0][:], tmp[1][:], op=ALU.add), reads=["br_t0", "br_t1"], writes=["br_t0"])
                    S.op("pool", lambda: nc.gpsimd.tensor_tensor(tmp[2][:], tmp[2][:], tmp[3][:], op=ALU.add), reads=["br_t2", "br_t3"], writes=["br_t2"])
                    S.op("pool", lambda: nc.gpsimd.tensor_tensor(y[:], tmp[0][:], tmp[2][:], op=ALU.add), reads=["br_t0", "br_t2"], writes=[yk])
                    S.dma("sp", R["YT"][c * 128:(c + 1) * 128, t0:t0 + 512], y[:], reads=[yk], writes=["YT"], nowaw=True)
            S.barrier()


def resid_epi(g, st, tag, xin, xout, xout_key, gate_ap, tok0):
    nc, S = g.nc, g.S
    xo = [g.sbt(st, "%s_xo%d" % (tag, i), [128, 512]) for i in range(3)]
    cnt = [0]

    def epi(c, ci, tt, p, pkey):
        i = cnt[0] % 3
        cnt[0] += 1
        b, k = xo[i], "%s_xo%d" % (tag, i)
        t0 = tok0 + tt * 512
        S.dma("sp", b[:], xin[c * 128:(c + 1) * 128, t0:t0 + 512], reads=["xin_" + tag], writes=[k])
        S.op("dve", lambda: nc.vector.scalar_tensor_tensor(b[:], p, gate_ap[:, c:c + 1], b[:], op0=ALU.mult, op1=ALU.add), reads=[pkey, k], writes=[k])
        S.dma("sp", xout[c * 128:(c + 1) * 128, t0:t0 + 512], b[:], reads=[k], writes=[xout_key], nowaw=True)
    return epi


def ph_wout(g, l, xin, xout):
    nc, S, I, R = g.nc, g.S, g.I, g.R
    L = str(l)
    TB = 2048
    Wo = I["w_out_" + L]
    chunks = [(Wo[:, c * 128:(c + 1) * 128], 128, c) for c in range(KC)]
    for tb in range(T // TB):
        with ExitStack() as st:
            yT = g.sbt(st, "wo_yT", [128, KC, TB], BF16)
            for k0 in range(0, KC, 8):
                S.dma("sp", yT[:, k0:k0 + 8, :], R["YT"][k0 * 128:(k0 + 8) * 128, tb * TB:(tb + 1) * TB].rearrange("(kc p) t -> p kc t", p=128),
                      reads=["YT"], writes=["wo_yT"], nowaw=(k0 > 0))
            epi = resid_epi(g, st, "wo", xin, xout, "xres%d" % (2 * l + 1), g.mod[l][:, 2 * KC:3 * KC], tb * TB)
            proj_fm(g, st, chunks, yT, "wo_yT", KC, TB, epi, tag="pwo")
            S.barrier()


def ph_ffn(g, l, xin, xout):
    nc, S, I, R = g.nc, g.S, g.I, g.R
    L = str(l)
    TB = 2048
    moe = (l % 2 == 1)
    if not moe:
        NF = D_FF // 128
        wg_of = lambda c: I["ffn_gate_0"][:, c * 128:(c + 1) * 128]
        wu_of = lambda c: I["ffn_up_0"][:, c * 128:(c + 1) * 128]
        Wd = I["ffn_down_0"]
    else:
        NF = N_EXP * D_FFE // 128
        CPE = D_FFE // 128
        wg_of = lambda c: I["expert_gate_1"][c // CPE][:, (c % CPE) * 128:(c % CPE + 1) * 128]
        wu_of = lambda c: I["expert_up_1"][c // CPE][:, (c % CPE) * 128:(c % CPE + 1) * 128]
        Wd = I["expert_down_1"]
    for tb in range(T // TB):
        with ExitStack() as st:
            hT = g.sbt(st, "ff_hT", [128, KC, TB], BF16)
            with ExitStack() as st2:
                extra = None
                if moe:
                    rt = g.sbt(st2, "ff_rt", [128, KC, N_EXP])
                    S.dma("sp", rt[:], I["router_1"].rearrange("(kc p) e -> p kc e", p=128), writes=["ff_rt"])
                    lps = g.pst(st2, "ff_lps", [128, 512])
                    tps = g.pst(st2, "ff_tps", [128, 512])
                    lg = g.sbt(st2, "ff_lg", [128, 8])
                    m8 = g.sbt(st2, "ff_m8", [128, 8])
                    nm1 = g.sbt(st2, "ff_nm1", [128, 1])
                    sel = g.sbt(st2, "ff_sel", [128, 8])
                    ew = g.sbt(st2, "ff_ew", [128, 8])
                    den = g.sbt(st2, "ff_den", [128, 1])
                    rwT = g.sbt(st2, "ff_rwT", [8, 128])

                    def extra(ti, hf, hfk):
                        t0 = tb * TB + ti * 128
                        for kc in range(KC):
                            S.op("pe", lambda: nc.tensor.matmul(lps[:, 0:8], hf[:, kc, :], rt[:, kc, :], start=(kc == 0), stop=(kc == KC - 1)),
                                 reads=[hfk, "ff_rt"], writes=["ff_lps"], sig=(kc == KC - 1))
                        S.op("dve", lambda: nc.vector.tensor_copy(lg[:], lps[:, 0:8]), reads=["ff_lps"], writes=["ff_lg"])
                        S.op("dve", lambda: nc.vector.max(out=m8[:], in_=lg[:]), reads=["ff_lg"], writes=["ff_m8"])
                        S.op("dve", lambda: nc.vector.tensor_scalar(nm1[:], m8[:, 0:1], -1.0, None, op0=ALU.mult), reads=["ff_m8"], writes=["ff_nm1"])
                        S.op("dve", lambda: nc.vector.tensor_scalar(sel[:], lg[:], m8[:, 1:2], None, op0=ALU.is_ge), reads=["ff_lg", "ff_m8"], writes=["ff_sel"])
                        S.op("act", lambda: nc.scalar.activation(ew[:], lg[:], AF.Exp, bias=nm1[:, 0:1], scale=1.0), reads=["ff_lg", "ff_nm1"], writes=["ff_ew"])
                        S.op("dve", lambda: nc.vector.tensor_tensor(ew[:], ew[:], sel[:], op=ALU.mult), reads=["ff_ew", "ff_sel"], writes=["ff_ew"])
                        S.op("dve", lambda: nc.vector.tensor_reduce(out=den[:], in_=ew[:], axis=AX.X, op=ALU.add), reads=["ff_ew"], writes=["ff_den"])
                        S.op("dve", lambda: nc.vector.reciprocal(den[:], den[:]), reads=["ff_den"], writes=["ff_den"])
                        S.op("dve", lambda: nc.vector.tensor_scalar(ew[:], ew[:], den[:, 0:1], None, op0=ALU.mult), reads=["ff_ew", "ff_den"], writes=["ff_ew"])
                        S.op("pe", lambda: nc.tensor.transpose(tps[0:8, 0:128], ew[:], g.identf[:]), reads=["ff_ew", "identf"], writes=["ff_tps"])
                        S.op("act", lambda: nc.scalar.copy(rwT[:], tps[0:8, 0:128]), reads=["ff_tps"], writes=["ff_rwT"])
                        S.dma("sp", R["RW"][:, t0:t0 + 128], rwT[:], reads=["ff_rwT"], writes=["RW"], nowaw=True)

                norm_block(g, st2, xin, tb * TB, TB, g.AB[l][:, KC:2 * KC], g.mod[l][:, 3 * KC:4 * KC], hT, "ff_hT", extra=extra, tag="nf")
                S.barrier()
            with ExitStack() as st2:
                wg = [g.sbt(st2, "ff_wg%d" % i, [128, KC, 128], BF16) for i in range(2)]
                wu = [g.sbt(st2, "ff_wu%d" % i, [128, KC, 128], BF16) for i in range(2)]
                pg = [g.pst(st2, "ff_pg%d" % i, [128, 512]) for i in range(2)]
                pu = [g.pst(st2, "ff_pu%d" % i, [128, 512]) for i in range(2)]
                sgb = [g.sbt(st2, "ff_sg%d" % i, [128, 512]) for i in range(2)]
                hb = [g.sbt(st2, "ff_hb%d" % i, [128, 512], BF16) for i in range(3)]
                hf32 = [g.sbt(st2, "ff_hf%d" % i, [128, 512]) for i in range(2)]
                rwb = g.sbt(st2, "ff_rwb", [128, TB]) if moe else None
                it = 0
                for c in range(NF):
                    bi = c % 2
                    S.dma("pool", wg[bi][:], wg_of(c).rearrange("(kc p) n -> p kc n", p=128), writes=["ff_wg%d" % bi])
                    S.dma("pool", wu[bi][:], wu_of(c).rearrange("(kc p) n -> p kc n", p=128), writes=["ff_wu%d" % bi])
                    if moe and c % CPE == 0:
                        e = c // CPE
                        S.dma("sp", rwb[:], R["RW"][e:e + 1, tb * TB:(tb + 1) * TB].to_broadcast([128, TB]), reads=["RW"], writes=["ff_rwb"])
                    for tt in range(TB // 512):
                        i2 = it % 2
                        i3 = it % 3
                        it += 1
                        for kc in range(KC):
                            S.op("pe", lambda: nc.tensor.matmul(pg[i2][:], wg[bi][:, kc, :], hT[:, kc, tt * 512:(tt + 1) * 512], start=(kc == 0), stop=(kc == KC - 1)),
                                 reads=["ff_wg%d" % bi, "ff_hT"], writes=["ff_pg%d" % i2], sig=(kc == KC - 1))
                        for kc in range(KC):
                            S.op("pe", lambda: nc.tensor.matmul(pu[i2][:], wu[bi][:, kc, :], hT[:, kc, tt * 512:(tt + 1) * 512], start=(kc == 0), stop=(kc == KC - 1)),
                                 reads=["ff_wu%d" % bi, "ff_hT"], writes=["ff_pu%d" % i2], sig=(kc == KC - 1))
                        S.op("act", lambda: nc.scalar.activation(sgb[i2][:], pg[i2][:], AF.Silu), reads=["ff_pg%d" % i2], writes=["ff_sg%d" % i2])
                        if not moe:
                            S.op("dve", lambda: nc.vector.tensor_tensor(hb[i3][:], sgb[i2][:], pu[i2][:], op=ALU.mult), reads=["ff_sg%d" % i2, "ff_pu%d" % i2], writes=["ff_hb%d" % i3])
                        else:
                            S.op("dve", lambda: nc.vector.tensor_tensor(hf32[i2][:], sgb[i2][:], pu[i2][:], op=ALU.mult), reads=["ff_sg%d" % i2, "ff_pu%d" % i2], writes=["ff_hf%d" % i2])
                            S.op("pool", lambda: nc.gpsimd.tensor_tensor(hb[i3][:], hf32[i2][:], rwb[:, tt * 512:(tt + 1) * 512], op=ALU.mult),
                                 reads=["ff_hf%d" % i2, "ff_rwb"], writes=["ff_hb%d" % i3])
                        t0 = tb * TB + tt * 512
                        S.dma("sp", R["HID"][c * 128:(c + 1) * 128, t0:t0 + 512], hb[i3][:], reads=["ff_hb%d" % i3], writes=["HID"], nowaw=True)
                S.barrier()
    FO = 256
    KG = 16
    with ExitStack() as st:
        wd = g.sbt(st, "fd_w", [128, NF, FO], BF16)
        hbuf = [g.sbt(st, "fd_h%d" % i, [128, KG, 512], BF16) for i in range(3)]
        ps = [g.pst(st, "fd_ps%d" % i, [128, 512]) for i in range(4)]
        epi = resid_epi(g, st, "fd", xin, xout, "xres%d" % (2 * l + 2), g.mod[l][:, 5 * KC:6 * KC], 0)
        Wdv = Wd.rearrange("(kc p) n -> p kc n", p=128)
        Hv = R["HID"].rearrange("(kc p) t -> p kc t", p=128)
        it = 0
        pi = 0
        for fb in range(D // FO):
            for k0 in range(0, NF, KG):
                S.dma("pool", wd[:, k0:k0 + KG, :], Wdv[:, k0:k0 + KG, fb * FO:(fb + 1) * FO], writes=["fd_w"])
            for tt in range(T // 512):
                pA = ps[(pi % 2) * 2]
                pB = ps[(pi % 2) * 2 + 1]
                kA = "fd_ps%d" % ((pi % 2) * 2)
                kB = "fd_ps%d" % ((pi % 2) * 2 + 1)
                pi += 1
                for kg in range(NF // KG):
                    hbf = hbuf[it % 3]
                    hk = "fd_h%d" % (it % 3)
                    it += 1
                    S.dma("sp", hbf[:], Hv[:, kg * KG:(kg + 1) * KG, tt * 512:(tt + 1) * 512], reads=["HID"], writes=[hk])
                    for k in range(KG):
                        kc = kg * KG + k
                        S.op("pe", lambda: nc.tensor.matmul(pA[:], wd[:, kc, 0:128], hbf[:, k, :], start=(kc == 0), stop=(kc == NF - 1)),
                             reads=["fd_w", hk], writes=[kA], sig=False)
                        S.op("pe", lambda: nc.tensor.matmul(pB[:], wd[:, kc, 128:256], hbf[:, k, :], start=(kc == 0), stop=(kc == NF - 1)),
                             reads=["fd_w", hk], writes=[kB], sig=(k == KG - 1))
                epi(fb * 2, 0, tt, pA[:], kA)
                epi(fb * 2 + 1, 0, tt, pB[:], kB)
        S.barrier()


def ph_final(g, xin, outT):
    nc, S = g.nc, g.S
    with ExitStack() as st:
        norm_block(g, st, xin, 0, T, g.fnorm, None, None, None, tag="nfin", out_dram=outT)
        S.barrier()


_CACHE = {}


def kernel(**inputs):
    nb = int(np.asarray(inputs["x"]).shape[0])
    if "nc" not in _CACHE:
        _CACHE["nc"] = build(layers=(0, 1))
    nc, g = _CACHE["nc"]
    sh = prep_shared(inputs, (0, 1))
    in_maps = []
    for b in range(nb):
        m = dict(sh)
        m.update(prep_core(inputs, b))
        in_maps.append({k: m[k] for k in g.I})
    res = run_bass_kernel_spmd(nc, in_maps, core_ids=list(range(nb)))
    out = np.stack([np.ascontiguousarray(np.asarray(res.results[b]["outT"], np.float32).T) for b in range(nb)], axis=0)
    return out
```

```python
import numpy as np
from contextlib import ExitStack
import concourse.bass as bass
import concourse.mybir as mybir
from concourse.bass_utils import run_bass_kernel_spmd

F32 = mybir.dt.float32
BF16 = mybir.dt.bfloat16
I32 = mybir.dt.int32
AF = mybir.ActivationFunctionType
ALU = mybir.AluOpType
AX = mybir.AxisListType

D = 4096
T = 4096
KC = D // 128
NT = T // 128
EPS = 1e-6
THETA = 500000.0
NEG = -1e30
OFF = {}
_sizes = [("a_q", 1536), ("a_k", 1536), ("a_v", 1536), ("b_cq", 1536), ("b_ckv", 512), ("b_kr", 64),
          ("c_q", 1024), ("c_k", 1024), ("c_v", 1024), ("d_q", 1024), ("d_k", 128), ("d_v", 128),
          ("d_iq", 2048), ("d_ik", 64), ("d_iw", 32), ("g", 16384)]
_o = 0
for _n, _s in _sizes:
    OFF[_n] = (_o, _s)
    _o += _s
D_IN = _o
YF_ROWS = {}
_o = 0
for _n, _s in [("a_q", 1536), ("a_k", 1536), ("b_cq", 1536), ("b_ckv", 512), ("kr2", 128), ("c_q", 1024), ("c_k", 1024),
               ("d_q", 1024), ("d_k", 128), ("d_iq", 2048), ("ik2", 128)]:
    YF_ROWS[_n] = _o
    _o += _s
YF_N = _o
D_FF = 14336
N_EXP = 8
D_FFE = 3072


class Sync:
    def __init__(self, nc, stack):
        self.nc = nc
        self.stack = stack
        self.engs = {"pe": nc.tensor, "act": nc.scalar, "dve": nc.vector, "pool": nc.gpsimd, "sp": nc.sync}
        self.sem = {}
        self.cnt = {}
        for e in ("pe", "act", "dve", "pool"):
            self.sem[e] = stack.enter_context(nc.semaphore("s_" + e))
            self.cnt[e] = 0
        self.seen = {e: {} for e in self.engs}
        self.last_write = {}
        self.readers = {}
        self.nsem = 0
        self.ninst = 0
        self.keysem = {}
        self.freesems = []

    def _wait(self, eng, tok):
        if tok is None:
            return
        name, val = tok
        if self.seen[eng].get(name, 0) >= val:
            return
        self.engs[eng].wait_ge(self.sem[name], val)
        self.seen[eng][name] = val

    def _record(self, tok, reads, writes):
        for k in writes:
            self.last_write[k] = tok
            self.readers[k] = {}
        for k in reads:
            d = self.readers.setdefault(k, {})
            if d.get(tok[0], 0) < tok[1]:
                d[tok[0]] = tok[1]

    def op(self, eng, fn, reads=(), writes=(), sig=True):
        selfsync = eng != "pe"

        def need(t):
            if t is None:
                return False
            if t[0] == eng:
                return selfsync and t[1] <= self.cnt[eng]
            return True
        for k in reads:
            t = self.last_write.get(k)
            if need(t):
                self._wait(eng, t)
        for k in writes:
            t = self.last_write.get(k)
            if need(t):
                self._wait(eng, t)
            for tok in self.readers.get(k, {}).items():
                if need(tok):
                    self._wait(eng, tok)
        inst = fn()
        self.ninst += 1
        if sig:
            self.cnt[eng] += 1
            inst.then_inc(self.sem[eng], 1)
            tok = (eng, self.cnt[eng])
        else:
            tok = (eng, self.cnt[eng] + 1)
        self._record(tok, reads, writes)
        return tok

    def dma(self, q, out, in_, reads=(), writes=(), nowaw=False, **kw):
        key = writes[0]
        if key not in self.keysem:
            if self.freesems:
                self.keysem[key] = self.freesems.pop()
            else:
                nm = "dsem%d" % self.nsem
                self.sem[nm] = self.stack.enter_context(self.nc.semaphore("sd%d" % self.nsem))
                self.nsem += 1
                self.cnt[nm] = 0
                self.keysem[key] = nm
        name = self.keysem[key]
        for k in reads:
            self._wait(q, self.last_write.get(k))
        for k in writes:
            if not nowaw:
                self._wait(q, self.last_write.get(k))
            for tok in self.readers.get(k, {}).items():
                self._wait(q, tok)
        inst = self.engs[q].dma_start(out=out, in_=in_, **kw)
        self.ninst += 1
        self.cnt[name] += 16
        inst.then_inc(self.sem[name], 16)
        tok = (name, self.cnt[name])
        self._record(tok, reads, writes)
        return tok

    def barrier(self):
        for e in self.engs:
            for name, c in self.cnt.items():
                if c > 0:
                    self._wait(e, (name, c))
        self.freesems.extend(self.keysem.values())
        self.keysem = {}
        self.last_write = {}
        self.readers = {}

    def wait_keys(self, eng, keys):
        for k in keys:
            self._wait(eng, self.last_write.get(k))


class Ctx:
    pass


def build(layers=(0, 1), stop=None, taps=()):
    nc = bass.Bass("TRN2", target_bir_lowering=False)
    top = ExitStack()
    g = Ctx()
    g.nc = nc
    with top:
        S = Sync(nc, top)
        g.S = S
        din = lambda n, s, dt=F32: nc.dram_tensor(n, list(s), dt, kind="ExternalInput").ap()
        dscr = lambda n, s, dt=F32: nc.dram_tensor(n, list(s), dt, kind="Internal").ap()
        shapes = {"xT": ([D, T], F32), "cc": ([128, KC], F32), "pos": ([1, T], I32), "w_ada": ([D, 6 * D], F32),
                  "b_ada": ([128, 6 * KC], F32), "freqs": ([128, 3], F32), "perm": ([3, 128, 128], F32),
                  "final_norm": ([128, KC], F32)}
        for l in (0, 1):
            L = str(l)
            shapes.update({"ada_table_" + L: ([128, 6 * KC], F32), "mix_norm_" + L: ([128, KC], F32), "w_in_" + L: ([D, D_IN], F32),
                           "w_kr2_" + L: ([D, 128], F32), "w_ik2_" + L: ([D, 128], F32), "mla_q_norm_" + L: ([128, 12], F32),
                           "mla_q_up_" + L: ([1536, 1536], F32), "mla_kv_norm_" + L: ([128, 4], F32), "mla_kv_up_" + L: ([512, 2048], F32),
                           "w_branch_" + L: ([3584, D], F32), "w_out_" + L: ([D, D], F32), "ffn_norm_" + L: ([128, KC], F32)})
        shapes.update({"ffn_gate_0": ([D, D_FF], F32), "ffn_up_0": ([D, D_FF], F32), "ffn_down_0": ([D_FF, D], F32),
                       "router_1": ([D, N_EXP], F32), "expert_gate_1": ([N_EXP, D, D_FFE], F32), "expert_up_1": ([N_EXP, D, D_FFE], F32),
                       "expert_down_1": ([N_EXP * D_FFE, D], F32)})

        class LazyIn(dict):
            def __missing__(self, k):
                shp, dt = shapes[k]
                v = nc.dram_tensor(k, list(shp), dt, kind="ExternalInput").ap()
                self[k] = v
                return v
        I = LazyIn()
        g.I = I
        outT = nc.dram_tensor("outT", [D, T], F32, kind="ExternalOutput").ap()
        R = {}
        R["YF"] = dscr("YF", [YF_N, T])
        R["G"] = dscr("G", [4 * D, T])
        R["VA"] = dscr("VA", [T, 1536], BF16)
        R["VC"] = dscr("VC", [T, 1024], BF16)
        R["VD"] = dscr("VD", [T, 128], BF16)
        R["IW"] = dscr("IW", [T, 32])
        R["VB"] = dscr("VB", [T, 1024], BF16)
        R["QB"] = dscr("QB", [1536, T], BF16)
        R["KB"] = dscr("KB", [1024, T], BF16)
        R["KPE"] = dscr("KPE", [128, T], BF16)
        R["IQR"] = dscr("IQR", [2048, T])
        R["OT"] = dscr("OT", [3584, T], BF16)
        R["YT"] = dscr("YT", [D, T], BF16)
        R["HID"] = dscr("HID", [N_EXP * D_FFE, T], BF16)
        R["CS"] = dscr("CS", [3 * 2 * 128, T])
        R["RW"] = dscr("RW", [N_EXP, T])
        R["X1"] = dscr("X1", [D, T])
        R["X2"] = dscr("X2", [D, T])
        R["X3"] = dscr("X3", [D, T])
        R["X4"] = dscr("X4", [D, T])
        R["X5"] = dscr("X5", [D, T])
        g.R = R
        tap_out = {}
        sb_taps = [(src, oname) for (src, oname) in taps if src.startswith("@")]
        taps = [(src, oname) for (src, oname) in taps if not src.startswith("@")]
        for (src, oname) in taps:
            a = R[src]
            tap_out[oname] = (src, nc.dram_tensor(oname, list(a.shape), a.dtype, kind="ExternalOutput").ap())
        g.uid = 0

        def sbt(st, n, s, dt=F32):
            g.uid += 1
            return st.enter_context(nc.sbuf_tensor("t%d_%s" % (g.uid, n), list(s), dt))
        def pst(st, n, s, dt=F32):
            g.uid += 1
            full = [128, 512] if dt == F32 else [128, 1024]
            return st.enter_context(nc.psum_tensor("t%d_%s" % (g.uid, n), full, dt))
        g.sbt, g.pst = sbt, pst
        g.ident = sbt(top, "ident", [128, 128], BF16)
        g.identf = sbt(top, "identf", [128, 128])
        g.ones = sbt(top, "ones", [128, 128], BF16)
        g.onesf = sbt(top, "onesf", [128, 128])
        g.mk_le = sbt(top, "mk_le", [128, 128], BF16)
        g.mk_ge = sbt(top, "mk_ge", [128, 128], BF16)
        g.negm = sbt(top, "negm", [128, 128])
        g.permT = sbt(top, "permT", [128, 3, 128])
        g.mod = {l: sbt(top, "mod%d" % l, [128, 6 * KC]) for l in layers}
        g.AB = {l: sbt(top, "AB%d" % l, [128, 4 * KC]) for l in layers}
        g.epsc = sbt(top, "epsc", [128, 1])
        g.fnorm = sbt(top, "fnorm", [128, KC])

        phases = []

        def phase(name, fn):
            phases.append((name, fn))

        phase("setup", lambda: ph_setup(g, layers))
        xs = [I["xT"], R["X1"], R["X2"], R["X3"], R["X4"]]
        for l in layers:
            phase("in%d" % l, lambda l=l: ph_mixer_in(g, l, xs[2 * l]))
            phase("mla%d" % l, lambda l=l: ph_mla(g, l))
            phase("moba%d" % l, lambda l=l: ph_moba(g, l))
            phase("dsa%d" % l, lambda l=l: ph_dsa(g, l))
            phase("dil%d" % l, lambda l=l: ph_dil(g, l))
            phase("branch%d" % l, lambda l=l: ph_branch(g, l))
            phase("wout%d" % l, lambda l=l: ph_wout(g, l, xs[2 * l], xs[2 * l + 1]))
            phase("ffn%d" % l, lambda l=l: ph_ffn(g, l, xs[2 * l + 1], xs[2 * l + 2]))
        phase("final", lambda: ph_final(g, xs[2 * len(layers)], outT))
        for name, fn in phases:
            fn()
            S.barrier()
            if stop == name:
                break
        for (src, oname) in sb_taps:
            tl = {"@mod0": g.mod.get(0), "@AB0": g.AB.get(0), "@mod1": g.mod.get(1), "@AB1": g.AB.get(1)}[src]
            o_ap = nc.dram_tensor(oname, list(tl.shape), F32, kind="ExternalOutput").ap()
            S.dma("sp", o_ap[:, :], tl[:], writes=["tap_" + oname])
        for oname, (src, dst) in tap_out.items():
            a = R[src]
            rows = a.shape[0]
            step = 128
            for r0 in range(0, rows, step):
                r1 = min(rows, r0 + step)
                S.dma("sp", dst[r0:r1, :], a[r0:r1, :], reads=[src], writes=["tap_" + oname], nowaw=True)
        S.barrier()
    g.ninst = S.ninst
    return nc, g


def ph_setup(g, layers):
    nc, S, I, R = g.nc, g.S, g.I, g.R
    with ExitStack() as st:
        S.op("pool", lambda: nc.gpsimd.memset(g.ident[:], 0.0), writes=["ident"])
        S.op("pool", lambda: nc.gpsimd.affine_select(g.ident[:], g.ident[:], pattern=[[-1, 128]], compare_op=ALU.not_equal,
                                                      fill=1.0, base=0, channel_multiplier=1), reads=["ident"], writes=["ident"])
        S.op("dve", lambda: nc.vector.tensor_copy(g.identf[:], g.ident[:]), reads=["ident"], writes=["identf"])
        S.op("pool", lambda: nc.gpsimd.memset(g.ones[:], 1.0), writes=["ones"])
        S.op("pool", lambda: nc.gpsimd.memset(g.onesf[:], 1.0), writes=["onesf"])
        S.op("pool", lambda: nc.gpsimd.memset(g.epsc[:], EPS), writes=["epsc"])
        S.op("pool", lambda: nc.gpsimd.memset(g.mk_le[:], 1.0), writes=["mk_le"])
        S.op("pool", lambda: nc.gpsimd.affine_select(g.mk_le[:], g.mk_le[:], pattern=[[1, 128]], compare_op=ALU.is_ge,
                                                      fill=0.0, base=0, channel_multiplier=-1), reads=["mk_le"], writes=["mk_le"])
        S.op("pool", lambda: nc.gpsimd.memset(g.mk_ge[:], 1.0), writes=["mk_ge"])
        S.op("pool", lambda: nc.gpsimd.affine_select(g.mk_ge[:], g.mk_ge[:], pattern=[[-1, 128]], compare_op=ALU.is_ge,
                                                      fill=0.0, base=0, channel_multiplier=1), reads=["mk_ge"], writes=["mk_ge"])
        S.op("pool", lambda: nc.gpsimd.memset(g.negm[:], 0.0), writes=["negm"])
        S.op("pool", lambda: nc.gpsimd.affine_select(g.negm[:], g.negm[:], pattern=[[-1, 128]], compare_op=ALU.is_ge,
                                                      fill=NEG, base=0, channel_multiplier=1), reads=["negm"], writes=["negm"])
        S.dma("sp", g.permT[:], I["perm"].rearrange("c k m -> k c m"), writes=["permT"])
        S.dma("sp", g.fnorm[:], I["final_norm"][:, :], writes=["fnorm"])
        posi = g.sbt(st, "posi", [128, T], I32)
        posf = g.sbt(st, "posf", [128, T])
        fr = g.sbt(st, "fr", [128, 3])
        th = g.sbt(st, "th", [128, T])
        ki = g.sbt(st, "ki", [128, T], I32)
        kf = g.sbt(st, "kf", [128, T])
        rr = g.sbt(st, "rr", [128, T])
        S.dma("sp", posi[:], I["pos"][0:1, :].to_broadcast([128, T]), writes=["posi"])
        S.dma("sp", fr[:], I["freqs"][:, :], writes=["fr"])
        S.op("dve", lambda: nc.vector.tensor_copy(posf[:], posi[:]), reads=["posi"], writes=["posf"])
        TWO_PI = float(2 * np.pi)
        for cfg in range(3):
            for which in range(2):
                shift = float(np.pi / 2) if which == 0 else 0.0
                S.op("dve", lambda: nc.vector.tensor_scalar(th[:], posf[:], fr[:, cfg:cfg + 1], shift, op0=ALU.mult, op1=ALU.add),
                     reads=["posf", "fr"], writes=["th"])
                S.op("dve", lambda: nc.vector.tensor_scalar(ki[:], th[:], 1.0 / TWO_PI, None, op0=ALU.mult), reads=["th"], writes=["ki"])
                S.op("dve", lambda: nc.vector.tensor_copy(kf[:], ki[:]), reads=["ki"], writes=["kf"])
                S.op("dve", lambda: nc.vector.scalar_tensor_tensor(rr[:], kf[:], -TWO_PI, th[:], op0=ALU.mult, op1=ALU.add),
                     reads=["kf", "th"], writes=["rr"])
                S.op("dve", lambda: nc.vector.tensor_scalar(rr[:], rr[:], float(np.pi), float(-np.pi), op0=ALU.min, op1=ALU.max),
                     reads=["rr"], writes=["rr"])
                S.op("act", lambda: nc.scalar.activation(th[:], rr[:], AF.Sin), reads=["rr"], writes=["th"])
                r0 = (cfg * 2 + which) * 128
                S.dma("sp", R["CS"][r0:r0 + 128, :], th[:], reads=["th"], writes=["CS"], nowaw=True)
        cc = g.sbt(st, "cc", [128, KC])
        sc = g.sbt(st, "sc", [128, KC])
        S.dma("sp", cc[:], I["cc"][:, :], writes=["cc"])
        S.op("act", lambda: nc.scalar.activation(sc[:], cc[:], AF.Silu), reads=["cc"], writes=["sc"])
        modps = g.pst(st, "modps", [128, 6 * KC])
        NB = 256
        wa = [g.sbt(st, "wa%d" % i, [128, KC, NB]) for i in range(2)]
        wv = I["w_ada"].rearrange("(kc p) n -> p kc n", p=128)
        for cb in range(6 * D // NB):
            buf = wa[cb % 2]
            key = "wa%d" % (cb % 2)
            S.dma("sp", buf[:, 0:16, :], wv[:, 0:16, cb * NB:(cb + 1) * NB], writes=[key])
            S.dma("sp", buf[:, 16:32, :], wv[:, 16:32, cb * NB:(cb + 1) * NB], writes=[key], nowaw=True)
            for j4 in range(NB // 128):
                j = cb * (NB // 128) + j4
                for kc in range(KC):
                    S.op("pe", lambda: nc.tensor.matmul(modps[:, j:j + 1], buf[:, kc, j4 * 128:(j4 + 1) * 128], sc[:, kc:kc + 1],
                                                       start=(kc == 0), stop=(kc == KC - 1)),
                         reads=[key, "sc"], writes=["modps"], sig=(kc == KC - 1))
        badd = g.sbt(st, "badd", [128, 6 * KC])
        S.dma("sp", badd[:], I["b_ada"][:, :], writes=["badd"])
        for l in layers:
            L = str(l)
            tab = g.sbt(st, "tab" + L, [128, 6 * KC])
            nrm = g.sbt(st, "nrm" + L, [128, 2 * KC])
            S.dma("sp", tab[:], I["ada_table_" + L][:, :], writes=["tab" + L])
            S.dma("sp", nrm[:, 0:KC], I["mix_norm_" + L][:, :], writes=["nrm" + L])
            S.dma("sp", nrm[:, KC:2 * KC], I["ffn_norm_" + L][:, :], reads=[], writes=["nrmb" + L])
            mk = "mod%d" % l
            S.op("dve", lambda: nc.vector.tensor_tensor(g.mod[l][:], modps[:, 0:6 * KC], badd[:], op=ALU.add), reads=["modps", "badd"], writes=[mk])
            S.op("dve", lambda: nc.vector.tensor_tensor(g.mod[l][:], g.mod[l][:], tab[:], op=ALU.add), reads=[mk, "tab" + L], writes=[mk])
            ak = "AB%d" % l
            S.op("dve", lambda: nc.vector.scalar_tensor_tensor(g.AB[l][:, 0:KC], g.mod[l][:, KC:2 * KC], 1.0, nrm[:, 0:KC], op0=ALU.add, op1=ALU.mult),
                 reads=[mk, "nrm" + L], writes=[ak])
            S.op("dve", lambda: nc.vector.scalar_tensor_tensor(g.AB[l][:, KC:2 * KC], g.mod[l][:, 4 * KC:5 * KC], 1.0, nrm[:, KC:2 * KC], op0=ALU.add, op1=ALU.mult),
                 reads=[mk, "nrmb" + L], writes=[ak])
        S.barrier()


def norm_block(g, st, src, t0, ntok, A_ap, B_ap, dst, dst_key, nkc=KC, tile=128, tag="nb", extra=None, out_dram=None):
    nc, S = g.nc, g.S
    srcv = src.rearrange("(kc p) t -> p kc t", p=128)
    xb = [g.sbt(st, "%s_x%d" % (tag, i), [128, nkc, tile]) for i in range(2)]
    sq = g.sbt(st, tag + "_sq", [128, nkc, tile])
    ssq = g.pst(st, tag + "_ssq", [128, tile])
    rstd = g.sbt(st, tag + "_rstd", [128, tile])
    hf = g.sbt(st, tag + "_hf", [128, nkc, tile]) if (extra is not None or out_dram is not None) else None
    nfeat = float(nkc * 128)
    for ti in range(ntok // tile):
        x = xb[ti % 2]
        xk = "%s_x%d" % (tag, ti % 2)
        S.dma("sp", x[:], srcv[:, :, t0 + ti * tile:t0 + (ti + 1) * tile], writes=[xk])
        S.op("act", lambda: nc.scalar.activation(sq[:], x[:], AF.Square), reads=[xk], writes=[tag + "_sq"])
        for kc in range(nkc):
            S.op("pe", lambda: nc.tensor.matmul(ssq[:, 0:tile], g.onesf[:], sq[:, kc, :], start=(kc == 0), stop=(kc == nkc - 1)),
                 reads=[tag + "_sq", "onesf"], writes=[tag + "_ssq"], sig=(kc == nkc - 1))
        S.op("act", lambda: nc.scalar.activation(rstd[:], ssq[:, 0:tile], AF.Sqrt, bias=g.epsc[:, 0:1], scale=1.0 / nfeat),
             reads=[tag + "_ssq", "epsc"], writes=[tag + "_rstd"])
        S.op("dve", lambda: nc.vector.reciprocal(rstd[:], rstd[:]), reads=[tag + "_rstd"], writes=[tag + "_rstd"])
        S.op("dve", lambda: nc.vector.tensor_tensor(sq[:], x[:], rstd[:, None, :].to_broadcast([128, nkc, tile]), op=ALU.mult),
             reads=[xk, tag + "_rstd"], writes=[tag + "_sq"])
        for kc in range(nkc):
            bias = B_ap[:, kc:kc + 1] if B_ap is not None else 0.0
            if dst is not None:
                S.op("act", lambda: nc.scalar.activation(dst[:, kc, ti * tile:(ti + 1) * tile], sq[:, kc, :], AF.Identity,
                                                         bias=bias, scale=A_ap[:, kc:kc + 1]),
                     reads=[tag + "_sq"], writes=[dst_key], sig=(kc == nkc - 1))
            if hf is not None:
                S.op("act", lambda: nc.scalar.activation(hf[:, kc, :], sq[:, kc, :], AF.Identity, bias=bias, scale=A_ap[:, kc:kc + 1]),
                     reads=[tag + "_sq"], writes=[tag + "_hf"], sig=(kc == nkc - 1))
        if extra is not None:
            extra(ti, hf, tag + "_hf")
        if out_dram is not None:
            S.dma("sp", out_dram.rearrange("(kc p) t -> p kc t", p=128)[:, :, t0 + ti * tile:t0 + (ti + 1) * tile], hf[:],
                  reads=[tag + "_hf"], writes=["out_" + tag], nowaw=True)


def proj_fm(g, st, chunks, act, act_key, nkc, ntok, epi, tag="pf", wq="pool", nbank=4):
    nc, S = g.nc, g.S
    GRP = 2
    wb = [g.sbt(st, "%s_w%d" % (tag, i), [128, nkc, GRP * 128], BF16) for i in range(2)]
    ps = [g.pst(st, "%s_ps%d" % (tag, i), [128, 512]) for i in range(nbank)]
    pcnt = 0
    for gi in range(0, len(chunks), GRP):
        grp = chunks[gi:gi + GRP]
        bi = (gi // GRP) % 2
        wkey = "%s_w%d" % (tag, bi)
        first = True
        for j, (w_ap, n, info) in enumerate(grp):
            S.dma(wq, wb[bi][:, :, j * 128:j * 128 + n], w_ap.rearrange("(kc p) n -> p kc n", p=128), writes=[wkey], nowaw=not first)
            first = False
        for j, (w_ap, n, info) in enumerate(grp):
            for tt in range(ntok // 512):
                p = ps[pcnt % nbank]
                pkey = "%s_ps%d" % (tag, pcnt % nbank)
                pcnt += 1
                for kc in range(nkc):
                    S.op("pe", lambda: nc.tensor.matmul(p[0:n, :], wb[bi][:, kc, j * 128:j * 128 + n], act[:, kc, tt * 512:(tt + 1) * 512],
                                                       start=(kc == 0), stop=(kc == nkc - 1)),
                         reads=[wkey, act_key], writes=[pkey], sig=(kc == nkc - 1))
                epi(info, gi + j, tt, p[0:n, :], pkey)


def proj_tm(g, st, wblocks, act, act_key, nkc, ntok, epi, tag="pt", wq="pool", nbank=2, wmax=512):
    nc, S = g.nc, g.S
    wb = [g.sbt(st, "%s_w%d" % (tag, i), [128, nkc, wmax], BF16) for i in range(2)]
    ps = [g.pst(st, "%s_ps%d" % (tag, i), [128, 512]) for i in range(nbank)]
    pcnt = 0
    for bi_, (w_ap, n, info) in enumerate(wblocks):
        bi = bi_ % 2
        wkey = "%s_w%d" % (tag, bi)
        S.dma(wq, wb[bi][:, :, 0:n], w_ap.rearrange("(kc p) n -> p kc n", p=128), writes=[wkey])
        for ti in range(ntok // 128):
            p = ps[pcnt % nbank]
            pkey = "%s_ps%d" % (tag, pcnt % nbank)
            pcnt += 1
            for kc in range(nkc):
                S.op("pe", lambda: nc.tensor.matmul(p[:, 0:n], act[:, kc, ti * 128:(ti + 1) * 128], wb[bi][:, kc, 0:n],
                                                   start=(kc == 0), stop=(kc == nkc - 1)),
                     reads=[wkey, act_key], writes=[pkey], sig=(kc == nkc - 1))
            epi(info, bi_, ti, p[:, 0:n], pkey)


class Evac:
    def __init__(self, g, st, tag, shape, dt, n=4):
        self.g = g
        self.bufs = [g.sbt(st, "%s_e%d" % (tag, i), shape, dt) for i in range(n)]
        self.keys = ["%s_e%d" % (tag, i) for i in range(n)]
        self.i = 0

    def next(self):
        b, k = self.bufs[self.i % len(self.bufs)], self.keys[self.i % len(self.bufs)]
        self.i += 1
        return b, k


def ph_mixer_in(g, l, xsrc):
    nc, S, I, R = g.nc, g.S, g.I, g.R
    L = str(l)
    W = I["w_in_" + L]
    TB = 2048
    fm = []

    def add_fm(w, c0, ncols, dst, drow0, kind):
        for c in range(0, ncols, 128):
            n = min(128, ncols - c)
            fm.append((w[:, c0 + c:c0 + c + n], n, (dst, drow0 + c, kind)))

    for nm in ("a_q", "a_k", "b_cq", "b_ckv", "c_q", "c_k", "d_q", "d_k", "d_iq"):
        add_fm(W, OFF[nm][0], OFF[nm][1], "YF", YF_ROWS[nm], "copy")
    add_fm(I["w_kr2_" + L], 0, 128, "YF", YF_ROWS["kr2"], "copy")
    add_fm(I["w_ik2_" + L], 0, 128, "YF", YF_ROWS["ik2"], "copy")
    add_fm(W, OFF["g"][0], OFF["g"][1], "G", 0, "sig")
    tm = []
    for nm, dst in (("a_v", "VA"), ("c_v", "VC"), ("d_v", "VD"), ("d_iw", "IW")):
        c0, ncols = OFF[nm]
        for c in range(0, ncols, 256):
            n = min(256, ncols - c)
            tm.append((W[:, c0 + c:c0 + c + n], n, (dst, c)))
    for tb in range(T // TB):
        with ExitStack() as st:
            hT = g.sbt(st, "hT", [128, KC, TB], BF16)
            with ExitStack() as st2:
                norm_block(g, st2, xsrc, tb * TB, TB, g.AB[l][:, 0:KC], g.mod[l][:, 0:KC], hT, "hT")
                S.barrier()
            with ExitStack() as st2:
                ev = Evac(g, st2, "ev", [128, 512], F32, 4)
                evb = Evac(g, st2, "evb", [128, 512], BF16, 2)

                def epi_fm(info, ci, tt, p, pkey):
                    dst, row, kind = info
                    n = p.shape[0]
                    b, k = ev.next()
                    func = AF.Sigmoid if kind == "sig" else AF.Copy
                    S.op("act", lambda: nc.scalar.activation(b[0:n, :], p, func), reads=[pkey], writes=[k])
                    S.dma("sp", R[dst][row:row + n, tb * TB + tt * 512: tb * TB + (tt + 1) * 512], b[0:n, :], reads=[k], writes=[dst], nowaw=True)

                def epi_tm(info, bi, ti, p, pkey):
                    dst, c = info
                    n = p.shape[1]
                    t0 = tb * TB + ti * 128
                    if dst == "IW":
                        b, k = ev.next()
                    else:
                        b, k = evb.next()
                    S.op("dve", lambda: nc.vector.tensor_copy(b[:, 0:n], p), reads=[pkey], writes=[k])
                    S.dma("sp", R[dst][t0:t0 + 128, c:c + n], b[:, 0:n], reads=[k], writes=[dst], nowaw=True)

                with ExitStack() as st3:
                    proj_tm(g, st3, tm, hT, "hT", KC, TB, epi_tm, wmax=256)
                    S.barrier()
                with ExitStack() as st3:
                    proj_fm(g, st3, fm, hT, "hT", KC, TB, epi_fm)
                    S.barrier()


def host_consts():
    freqs = np.zeros((128, 3), np.float32)
    perm = np.zeros((3, 128, 128), np.float32)
    th = np.float32(THETA)

    def fr(half, rot):
        return (th ** (-(np.arange(half, dtype=np.float32) * np.float32(2.0 / rot)))).astype(np.float32)
    f = fr(16, 32)
    for p in range(32):
        freqs[p, 0] = f[p % 16]
    for m in range(16):
        perm[0][m + 16, m] = -1.0
        perm[0][m, m + 16] = 1.0
    f = fr(8, 16)
    for base in (0, 64):
        for p in range(16):
            freqs[base + p, 1] = f[p % 8]
        for m in range(8):
            perm[1][base + m + 8, base + m] = -1.0
            perm[1][base + m, base + m + 8] = 1.0
    f = fr(32, 64)
    for base in (0, 64):
        for p in range(64):
            freqs[base + p, 2] = f[p % 32]
        for m in range(32):
            perm[2][base + m + 32, base + m] = -1.0
            perm[2][base + m, base + m + 32] = 1.0
    return freqs, perm


def colvec(v, n=None):
    v = np.asarray(v, np.float32)
    return np.ascontiguousarray(v.reshape(-1, 128).T)


def prep_shared(inputs, layers=(0, 1)):
    m = {}
    freqs, perm = host_consts()
    m["freqs"], m["perm"] = freqs, perm
    m["w_ada"] = np.asarray(inputs["w_ada"], np.float32)
    m["b_ada"] = colvec(inputs["b_ada"])
    m["final_norm"] = colvec(inputs["final_norm"])
    qperm = np.array([h * 192 + j for h in range(8) for j in range(128)] + [h * 192 + 128 + j for h in range(8) for j in range(64)])
    kvperm = np.array([h * 256 + j for h in range(8) for j in range(128)] + [h * 256 + 128 + j for h in range(8) for j in range(128)])
    for l in layers:
        L = str(l)
        m["ada_table_" + L] = np.ascontiguousarray(np.asarray(inputs["ada_table_" + L], np.float32).reshape(6 * KC, 128).T)
        m["mix_norm_" + L] = colvec(inputs["mix_norm_" + L])
        w_in = np.asarray(inputs["w_in_" + L], np.float32)
        m["w_in_" + L] = w_in
        kr = w_in[:, OFF["b_kr"][0]:OFF["b_kr"][0] + 64]
        ik = w_in[:, OFF["d_ik"][0]:OFF["d_ik"][0] + 64]
        m["w_kr2_" + L] = np.ascontiguousarray(np.concatenate([kr, kr], axis=1))
        m["w_ik2_" + L] = np.ascontiguousarray(np.concatenate([ik, ik], axis=1))
        m["mla_q_norm_" + L] = colvec(inputs["mla_q_norm_" + L])
        m["mla_q_up_" + L] = np.ascontiguousarray(np.asarray(inputs["mla_q_up_" + L], np.float32)[:, qperm])
        m["mla_kv_norm_" + L] = colvec(inputs["mla_kv_norm_" + L])
        m["mla_kv_up_" + L] = np.ascontiguousarray(np.asarray(inputs["mla_kv_up_" + L], np.float32)[:, kvperm])
        m["w_branch_" + L] = np.asarray(inputs["w_branch_" + L], np.float32)
        m["w_out_" + L] = np.asarray(inputs["w_out_" + L], np.float32)
        m["ffn_norm_" + L] = colvec(inputs["ffn_norm_" + L])
        if l == 0:
            for k in ("ffn_gate_0", "ffn_up_0", "ffn_down_0"):
                m[k] = np.asarray(inputs[k], np.float32)
        else:
            m["router_1"] = np.asarray(inputs["router_1"], np.float32)
            m["expert_gate_1"] = np.asarray(inputs["expert_gate_1"], np.float32)
            m["expert_up_1"] = np.asarray(inputs["expert_up_1"], np.float32)
            m["expert_down_1"] = np.asarray(inputs["expert_down_1"], np.float32).reshape(N_EXP * D_FFE, D)
    return m


def prep_core(inputs, b):
    m = {}
    m["xT"] = np.ascontiguousarray(np.asarray(inputs["x"][b], np.float32).T)
    m["cc"] = colvec(inputs["c"][b])
    m["pos"] = np.ascontiguousarray(np.asarray(inputs["positions"][b], np.int32)[None, :])
    return m


class Rope:
    def __init__(self, g, st, cfg, tag):
        nc, S, R = g.nc, g.S, g.R
        self.g, self.cfg, self.tag = g, cfg, tag
        self.C = g.sbt(st, tag + "_C", [128, T])
        self.Sn = g.sbt(st, tag + "_S", [128, T])
        S.dma("sp", self.C[:], R["CS"][(cfg * 2) * 128:(cfg * 2 + 1) * 128, :], reads=["CS"], writes=[tag + "_C"])
        S.dma("sp", self.Sn[:], R["CS"][(cfg * 2 + 1) * 128:(cfg * 2 + 2) * 128, :], reads=["CS"], writes=[tag + "_S"])
        self.t1 = [g.sbt(st, "%s_t1%d" % (tag, i), [128, 512]) for i in range(2)]
        self.t2 = [g.sbt(st, "%s_t2%d" % (tag, i), [128, 512]) for i in range(2)]
        self.ps = [g.pst(st, "%s_ps%d" % (tag, i), [128, 512]) for i in range(2)]
        self.i = 0

    def apply(self, x_ap, x_key, t0, out_ap, out_key):
        g = self.g
        nc, S = g.nc, g.S
        i = self.i % 2
        self.i += 1
        tag = self.tag
        ps, t1, t2 = self.ps[i], self.t1[i], self.t2[i]
        pk, k1, k2 = "%s_ps%d" % (tag, i), "%s_t1%d" % (tag, i), "%s_t2%d" % (tag, i)
        S.op("pe", lambda: nc.tensor.matmul(ps[:], g.permT[:, self.cfg, :], x_ap, start=True, stop=True), reads=[x_key, "permT"], writes=[pk])
        S.op("dve", lambda: nc.vector.tensor_tensor(t1[:], x_ap, self.C[:, t0:t0 + 512], op=ALU.mult), reads=[x_key, tag + "_C"], writes=[k1])
        S.op("dve", lambda: nc.vector.tensor_tensor(t2[:], ps[:], self.Sn[:, t0:t0 + 512], op=ALU.mult), reads=[pk, tag + "_S"], writes=[k2])
        S.op("pool", lambda: nc.gpsimd.tensor_tensor(out_ap, t1[:], t2[:], op=ALU.add), reads=[k1, k2], writes=[out_key])


def rope_rows(g, st, rope, src_rows, dst_dram, dst_key, tag, dst_dt=BF16):
    nc, S = g.nc, g.S
    x = g.sbt(st, tag + "_x", [128, T])
    o = g.sbt(st, tag + "_o", [128, T], dst_dt)
    S.dma("sp", x[:], src_rows, reads=["YF"], writes=[tag + "_x"])
    for tt in range(T // 512):
        rope.apply(x[:, tt * 512:(tt + 1) * 512], tag + "_x", tt * 512, o[:, tt * 512:(tt + 1) * 512], tag + "_o")
    S.dma("sp", dst_dram, o[:], reads=[tag + "_o"], writes=[dst_key], nowaw=True)


def ph_mla(g, l):
    nc, S, I, R = g.nc, g.S, g.I, g.R
    L = str(l)
    with ExitStack() as st:
        qn = g.sbt(st, "qnrm", [128, 12])
        S.dma("sp", qn[:], I["mla_q_norm_" + L][:, :], writes=["qnrm"])
        cqn = g.sbt(st, "cqn", [128, 12, T], BF16)
        with ExitStack() as st2:
            norm_block(g, st2, R["YF"][YF_ROWS["b_cq"]:YF_ROWS["b_cq"] + 1536, :], 0, T, qn, None, cqn, "cqn", nkc=12, tile=256, tag="nq")
            S.barrier()
        with ExitStack() as st2:
            rope = Rope(g, st2, 2, "rq")
            ev = Evac(g, st2, "evq", [128, 512], BF16, 3)
            evf = Evac(g, st2, "evqf", [128, 512], F32, 2)
            Wq = I["mla_q_up_" + L]
            chunks = [(Wq[:, c * 128:(c + 1) * 128], 128, c) for c in range(12)]

            def epi(c, ci, tt, p, pkey):
                b, k = ev.next()
                if c < 8:
                    S.op("act", lambda: nc.scalar.copy(b[:], p), reads=[pkey], writes=[k])
                else:
                    xf, xk = evf.next()
                    S.op("act", lambda: nc.scalar.copy(xf[:], p), reads=[pkey], writes=[xk])
                    rope.apply(xf[:], xk, tt * 512, b[:], k)
                S.dma("sp", R["QB"][c * 128:(c + 1) * 128, tt * 512:(tt + 1) * 512], b[:], reads=[k], writes=["QB"], nowaw=True)

            proj_fm(g, st2, chunks, cqn, "cqn", 12, T, epi, tag="pq")
            S.barrier()
    with ExitStack() as st:
        kn = g.sbt(st, "kvnrm", [128, 4])
        S.dma("sp", kn[:], I["mla_kv_norm_" + L][:, :], writes=["kvnrm"])
        ckvn = g.sbt(st, "ckvn", [128, 4, T], BF16)
        with ExitStack() as st2:
            norm_block(g, st2, R["YF"][YF_ROWS["b_ckv"]:YF_ROWS["b_ckv"] + 512, :], 0, T, kn, None, ckvn, "ckvn", nkc=4, tile=512, tag="nk")
            S.barrier()
        with ExitStack() as st2:
            ev = Evac(g, st2, "evk", [128, 512], BF16, 4)
            Wkv = I["mla_kv_up_" + L]
            chunks = [(Wkv[:, c * 128:(c + 1) * 128], 128, c) for c in range(8)]

            def epi(c, ci, tt, p, pkey):
                b, k = ev.next()
                S.op("act", lambda: nc.scalar.copy(b[:], p), reads=[pkey], writes=[k])
                S.dma("sp", R["KB"][c * 128:(c + 1) * 128, tt * 512:(tt + 1) * 512], b[:], reads=[k], writes=["KB"], nowaw=True)

            proj_fm(g, st2, chunks, ckvn, "ckvn", 4, T, epi, tag="pk")
            blocks = [(Wkv[:, 1024 + c * 512:1024 + (c + 1) * 512], 512, c) for c in range(2)]

            def epi_v(c, bi, ti, p, pkey):
                b, k = ev.next()
                S.op("dve", lambda: nc.vector.tensor_copy(b[:], p), reads=[pkey], writes=[k])
                S.dma("sp", R["VB"][ti * 128:(ti + 1) * 128, c * 512:(c + 1) * 512], b[:], reads=[k], writes=["VB"], nowaw=True)

            proj_tm(g, st2, blocks, ckvn, "ckvn", 4, T, epi_v, tag="pv")
            S.barrier()
    with ExitStack() as st:
        rope = Rope(g, st, 2, "rk")
        rope_rows(g, st, rope, R["YF"][YF_ROWS["kr2"]:YF_ROWS["kr2"] + 128, :], R["KPE"][:, :], "KPE", "rkr")
        S.barrier()
    scale = float((128 + 64) ** -0.5)
    with ExitStack() as st:
        kp = g.sbt(st, "kp", [128, T], BF16)
        S.dma("sp", kp[:], R["KPE"][:, :], reads=["KPE"], writes=["kp"])
        bufs = [{n: g.sbt(st, "%s%d" % (n, i), [128, T], BF16) for n in ("qn", "qp", "kn")} for i in range(2)]
        vb = [g.sbt(st, "vh%d" % i, [128, NT, 128], BF16) for i in range(2)]
        sps = [g.pst(st, "sps%d" % i, [128, 512]) for i in range(2)]
        ops_ = g.pst(st, "ops", [128, 512])
        dps = g.pst(st, "dps", [128, 512])
        pts = [g.sbt(st, "pt%d" % i, [128, 512], BF16) for i in range(3)]
        rd = g.sbt(st, "rd", [128, 512])
        ob = [g.sbt(st, "ob%d" % i, [128, 512], BF16) for i in range(2)]
        it = 0
        oi = 0
        for h in range(8):
            bi = h % 2
            B = bufs[bi]
            S.dma("sp", B["qn"][:], R["QB"][h * 128:(h + 1) * 128, :], reads=["QB"], writes=["qn%d" % bi])
            S.dma("sp", B["qp"][:], R["QB"][1024 + (h // 2) * 128:1024 + (h // 2 + 1) * 128, :], reads=["QB"], writes=["qp%d" % bi])
            S.dma("sp", B["kn"][:], R["KB"][h * 128:(h + 1) * 128, :], reads=["KB"], writes=["kn%d" % bi])
            S.dma("sp", vb[bi][:], R["VB"][:, h * 128:(h + 1) * 128].rearrange("(n p) c -> p n c", p=128), reads=["VB"], writes=["vh%d" % bi])
            pb = 64 * (h % 2)
            for qb in range(T // 512):
                q0 = qb * 512
                nkt = 4 * qb + 4
                for kt in range(nkt):
                    i = kt - 4 * qb
                    c0 = max(i, 0) * 128
                    sp_ = sps[it % 2]
                    sk = "sps%d" % (it % 2)
                    pt = pts[it % 3]
                    pk = "pt%d" % (it % 3)
                    it += 1
                    S.op("pe", lambda: nc.tensor.matmul(sp_[:, c0:512], B["kn"][:, kt * 128:(kt + 1) * 128], B["qn"][:, q0 + c0:q0 + 512], start=True, stop=False),
                         reads=["kn%d" % bi, "qn%d" % bi], writes=[sk], sig=False)
                    S.op("pe", lambda: nc.tensor.matmul(sp_[:, c0:512], kp[pb:pb + 64, kt * 128:(kt + 1) * 128], B["qp"][pb:pb + 64, q0 + c0:q0 + 512], start=False, stop=True),
                         reads=["kp", "qp%d" % bi], writes=[sk])
                    S.op("act", lambda: nc.scalar.activation(pt[:, c0:512], sp_[:, c0:512], AF.Exp, scale=scale), reads=[sk], writes=[pk])
                    if i >= 0:
                        S.op("dve", lambda: nc.vector.tensor_tensor(pt[:, c0:c0 + 128], pt[:, c0:c0 + 128], g.mk_le[:], op=ALU.mult), reads=[pk, "mk_le"], writes=[pk])
                    S.op("pe", lambda: nc.tensor.matmul(ops_[:, c0:512], vb[bi][:, kt, :], pt[:, c0:512], start=(kt == 0), stop=(kt == nkt - 1)),
                         reads=["vh%d" % bi, pk], writes=["ops"], sig=False)
                    S.op("pe", lambda: nc.tensor.matmul(dps[:, c0:512], g.ones[:], pt[:, c0:512], start=(kt == 0), stop=(kt == nkt - 1)),
                         reads=["ones", pk], writes=["dps"])
                o = ob[oi % 2]
                ok = "ob%d" % (oi % 2)
                oi += 1
                S.op("dve", lambda: nc.vector.reciprocal(rd[:], dps[:]), reads=["dps"], writes=["rd"])
                S.op("dve", lambda: nc.vector.tensor_tensor(o[:], ops_[:], rd[:], op=ALU.mult), reads=["ops", "rd"], writes=[ok])
                S.dma("sp", R["OT"][512 + h * 128:512 + (h + 1) * 128, q0:q0 + 512], o[:], reads=[ok], writes=["OT"], nowaw=True)
        S.barrier()


def build_maskT(g, Mq, mq_key, nkt, MT, mt_key, trp, trp_key):
    nc, S = g.nc, g.S
    for k0 in range(0, nkt, 4):
        n = min(4, nkt - k0)
        for j in range(n):
            S.op("pe", lambda: nc.tensor.transpose(trp[:, j * 128:(j + 1) * 128], Mq[:, (k0 + j) * 128:(k0 + j + 1) * 128], g.ident[:]),
                 reads=[mq_key, "ident"], writes=[trp_key], sig=(j == n - 1))
        S.op("act", lambda: nc.scalar.copy(MT[:, k0:k0 + n, :], trp[:, 0:n * 128].rearrange("p (a b) -> p a b", b=128)), reads=[trp_key], writes=[mt_key])


def ph_moba(g, l):
    nc, S, I, R = g.nc, g.S, g.I, g.R
    scale = float(128 ** -0.5)
    with ExitStack() as st:
        rope = Rope(g, st, 0, "rc")
        xin = g.sbt(st, "mb_x", [128, T])
        qf = g.sbt(st, "mb_qf", [128, T])
        kf = g.sbt(st, "mb_kf", [128, T])
        qb = g.sbt(st, "mb_qb", [128, T], BF16)
        kb = g.sbt(st, "mb_kb", [128, T], BF16)
        vv = g.sbt(st, "mb_v", [128, NT, 128], BF16)
        kmean = g.sbt(st, "mb_km", [128, 16])
        gm = g.sbt(st, "mb_gm", [128, 16])
        m8 = g.sbt(st, "mb_m8", [128, 8])
        sel = g.sbt(st, "mb_sel", [128, 16])
        Mq = g.sbt(st, "mb_Mq", [128, T], BF16)
        MT = g.sbt(st, "mb_MT", [128, NT, 128], BF16)
        gps = g.pst(st, "mb_gps", [128, 16])
        trp = g.pst(st, "mb_trp", [128, 512], BF16)
        sps = [g.pst(st, "mb_sps%d" % i, [128, 512]) for i in range(2)]
        ops_ = g.pst(st, "mb_ops", [128, 128])
        dps = g.pst(st, "mb_dps", [128, 128])
        pts = [g.sbt(st, "mb_pt%d" % i, [128, 512], BF16) for i in range(2)]
        rd = g.sbt(st, "mb_rd", [128, 128])
        ob = [g.sbt(st, "mb_ob%d" % i, [128, 512], BF16) for i in range(2)]
        it = 0
        for h in range(8):
            for (nm, dstf, dstb, fk, bk) in (("c_q", qf, qb, "mb_qf", "mb_qb"), ("c_k", kf, kb, "mb_kf", "mb_kb")):
                r0 = YF_ROWS[nm] + h * 128
                S.dma("sp", xin[:], R["YF"][r0:r0 + 128, :], reads=["YF"], writes=["mb_x"])
                for tt in range(T // 512):
                    rope.apply(xin[:, tt * 512:(tt + 1) * 512], "mb_x", tt * 512, dstf[:, tt * 512:(tt + 1) * 512], fk)
                S.op("act", lambda: nc.scalar.copy(dstb[:], dstf[:]), reads=[fk], writes=[bk])
            S.dma("sp", vv[:], R["VC"][:, h * 128:(h + 1) * 128].rearrange("(n p) c -> p n c", p=128), reads=["VC"], writes=["mb_v"])
            S.op("dve", lambda: nc.vector.tensor_reduce(out=kmean[:], in_=kf[:].rearrange("p (a b) -> p a b", b=256), axis=AX.X, op=ALU.add),
                 reads=["mb_kf"], writes=["mb_km"])
            S.op("dve", lambda: nc.vector.tensor_scalar(kmean[:], kmean[:], 1.0 / 256.0, None, op0=ALU.mult), reads=["mb_km"], writes=["mb_km"])
            S.op("dve", lambda: nc.vector.memset(gm[:], NEG), writes=["mb_gm"])
            for i in range(NT):
                own = i // 2
                nkt = i + 1
                if own > 3:
                    S.op("pe", lambda: nc.tensor.matmul(gps[:, 0:16], qf[:, i * 128:(i + 1) * 128], kmean[:], start=True, stop=True),
                         reads=["mb_qf", "mb_km"], writes=["mb_gps"])
                    S.op("dve", lambda: nc.vector.tensor_copy(gm[:, 0:own], gps[:, 0:own]), reads=["mb_gps"], writes=["mb_gm"])
                    S.op("dve", lambda: nc.vector.max(out=m8[:], in_=gm[:]), reads=["mb_gm"], writes=["mb_m8"])
                    S.op("dve", lambda: nc.vector.tensor_scalar(sel[:], gm[:], m8[:, 2:3], None, op0=ALU.is_ge), reads=["mb_gm", "mb_m8"], writes=["mb_sel"])
                    S.op("dve", lambda: nc.vector.tensor_copy(Mq[:, 0:own * 256].rearrange("p (a b) -> p a b", b=256),
                                                              sel[:, 0:own, None].to_broadcast([128, own, 256])), reads=["mb_sel"], writes=["mb_Mq"])
                elif own > 0:
                    S.op("dve", lambda: nc.vector.memset(Mq[:, 0:own * 256], 1.0), writes=["mb_Mq"])
                if i % 2 == 1:
                    S.op("dve", lambda: nc.vector.memset(Mq[:, own * 256:own * 256 + 128], 1.0), writes=["mb_Mq"])
                S.op("dve", lambda: nc.vector.tensor_copy(Mq[:, i * 128:(i + 1) * 128], g.mk_ge[:]), reads=["mk_ge"], writes=["mb_Mq"])
                build_maskT(g, Mq, "mb_Mq", nkt, MT, "mb_MT", trp, "mb_trp")
                for k0 in range(0, nkt, 4):
                    n = min(4, nkt - k0)
                    sp_ = sps[it % 2]
                    sk = "mb_sps%d" % (it % 2)
                    pt = pts[it % 2]
                    pk = "mb_pt%d" % (it % 2)
                    it += 1
                    for j in range(n):
                        S.op("pe", lambda: nc.tensor.matmul(sp_[:, j * 128:(j + 1) * 128], kb[:, (k0 + j) * 128:(k0 + j + 1) * 128], qb[:, i * 128:(i + 1) * 128],
                                                           start=True, stop=True), reads=["mb_kb", "mb_qb"], writes=[sk], sig=(j == n - 1))
                    S.op("act", lambda: nc.scalar.activation(pt[:, 0:n * 128], sp_[:, 0:n * 128], AF.Exp, scale=scale), reads=[sk], writes=[pk])
                    S.op("dve", lambda: nc.vector.tensor_tensor(pt[:, 0:n * 128], pt[:, 0:n * 128], MT[:, k0:k0 + n, :].rearrange("p a b -> p (a b)"), op=ALU.mult),
                         reads=[pk, "mb_MT"], writes=[pk])
                    for j in range(n):
                        kt = k0 + j
                        S.op("pe", lambda: nc.tensor.matmul(ops_[:, 0:128], vv[:, kt, :], pt[:, j * 128:(j + 1) * 128], start=(kt == 0), stop=(kt == nkt - 1)),
                             reads=["mb_v", pk], writes=["mb_ops"], sig=False)
                        S.op("pe", lambda: nc.tensor.matmul(dps[:, 0:128], g.ones[:], pt[:, j * 128:(j + 1) * 128], start=(kt == 0), stop=(kt == nkt - 1)),
                             reads=["ones", pk], writes=["mb_dps"], sig=(j == n - 1))
                o = ob[(i // 4) % 2]
                ok = "mb_ob%d" % ((i // 4) % 2)
                S.op("dve", lambda: nc.vector.reciprocal(rd[:], dps[:, 0:128]), reads=["mb_dps"], writes=["mb_rd"])
                S.op("dve", lambda: nc.vector.tensor_tensor(o[:, (i % 4) * 128:(i % 4 + 1) * 128], ops_[:, 0:128], rd[:], op=ALU.mult), reads=["mb_ops", "mb_rd"], writes=[ok])
                if i % 4 == 3:
                    q0 = (i // 4) * 512
                    S.dma("sp", R["OT"][1536 + h * 128:1536 + (h + 1) * 128, q0:q0 + 512], o[:], reads=[ok], writes=["OT"], nowaw=True)
        S.barrier()


def ph_dsa(g, l):
    nc, S, I, R = g.nc, g.S, g.I, g.R
    scale = float(128 ** -0.5)
    Qd = None
    with ExitStack() as st:
        Qd = g.sbt(st, "ds_Q", [128, 8, T], BF16)
        Kd = g.sbt(st, "ds_K", [128, T], BF16)
        ikf = g.sbt(st, "ds_ik", [128, T])
        Vd = g.sbt(st, "ds_V", [128, NT, 128], BF16)
        with ExitStack() as st2:
            rope0 = Rope(g, st2, 0, "rd0")
            xin = g.sbt(st2, "ds_x", [128, T])
            for h in range(9):
                r0 = YF_ROWS["d_q"] + h * 128 if h < 8 else YF_ROWS["d_k"]
                S.dma("sp", xin[:], R["YF"][r0:r0 + 128, :], reads=["YF"], writes=["ds_x"])
                for tt in range(T // 512):
                    dst = Qd[:, h, tt * 512:(tt + 1) * 512] if h < 8 else Kd[:, tt * 512:(tt + 1) * 512]
                    rope0.apply(xin[:, tt * 512:(tt + 1) * 512], "ds_x", tt * 512, dst, "ds_Q" if h < 8 else "ds_K")
            S.barrier()
        with ExitStack() as st2:
            rope1 = Rope(g, st2, 1, "rd1")
            xin = g.sbt(st2, "ds_x2", [128, T])
            xo = g.sbt(st2, "ds_xo", [128, T])
            for c in range(17):
                r0 = YF_ROWS["d_iq"] + c * 128 if c < 16 else YF_ROWS["ik2"]
                S.dma("sp", xin[:], R["YF"][r0:r0 + 128, :], reads=["YF"], writes=["ds_x2"])
                dstt, dk = (xo, "ds_xo") if c < 16 else (ikf, "ds_ik")
                for tt in range(T // 512):
                    rope1.apply(xin[:, tt * 512:(tt + 1) * 512], "ds_x2", tt * 512, dstt[:, tt * 512:(tt + 1) * 512], dk)
                if c < 16:
                    S.dma("sp", R["IQR"][c * 128:(c + 1) * 128, :], xo[:], reads=["ds_xo"], writes=["IQR"], nowaw=True)
            S.barrier()
        S.dma("sp", Vd[:], R["VD"][:, :].rearrange("(n p) c -> p n c", p=128), reads=["VD"], writes=["ds_V"])
        iqt = [g.sbt(st, "ds_iq%d" % i, [128, 16, 128]) for i in range(2)]
        iwt = g.sbt(st, "ds_iw", [128, 32])
        aw = g.sbt(st, "ds_aw", [128, 32])
        sg = g.sbt(st, "ds_sg", [128, 32])
        acc = g.sbt(st, "ds_acc", [128, T])
        wk = g.sbt(st, "ds_wk", [128, T])
        rl = [g.sbt(st, "ds_rl%d" % i, [128, 512]) for i in range(2)]
        m8 = g.sbt(st, "ds_m8", [128, 8])
        Mq = g.sbt(st, "ds_Mq", [128, T], BF16)
        MT = g.sbt(st, "ds_MT", [128, NT, 128], BF16)
        ips = [g.pst(st, "ds_ips%d" % i, [128, 512]) for i in range(2)]
        trp = g.pst(st, "ds_trp", [128, 512], BF16)
        sps = [g.pst(st, "ds_sps%d" % i, [128, 512]) for i in range(2)]
        ops_ = g.pst(st, "ds_ops", [128, 512])
        dps = g.pst(st, "ds_dps", [128, 512])
        pts = [g.sbt(st, "ds_pt%d" % i, [128, 512], BF16) for i in range(2)]
        rd = g.sbt(st, "ds_rd", [128, 512])
        ob = [g.sbt(st, "ds_ob%d" % i, [128, 4, 128], BF16) for i in range(2)]
        cidx = float((32 ** -0.5) * (64 ** -0.5))
        it = 0
        ii = 0
        oi = 0
        for i in range(NT):
            nkt = i + 1
            ns = nkt * 128
            if i >= 2:
                iq = iqt[i % 2]
                iqk = "ds_iq%d" % (i % 2)
                S.dma("sp", iq[:], R["IQR"][:, i * 128:(i + 1) * 128].rearrange("(c p) t -> p c t", p=128), reads=["IQR"], writes=[iqk])
                S.dma("sp", iwt[:], R["IW"][i * 128:(i + 1) * 128, :], reads=["IW"], writes=["ds_iw"])
                S.op("act", lambda: nc.scalar.activation(aw[:], iwt[:], AF.Abs, scale=cidx), reads=["ds_iw"], writes=["ds_aw"])
                S.op("act", lambda: nc.scalar.activation(sg[:], iwt[:], AF.Sign), reads=["ds_iw"], writes=["ds_sg"])
                for hh in range(32):
                    pb = 64 * (hh % 2)
                    for s0 in range(0, ns, 512):
                        n = min(512, ns - s0)
                        ip = ips[ii % 2]
                        ik_ = "ds_ips%d" % (ii % 2)
                        r = rl[ii % 2]
                        rk = "ds_rl%d" % (ii % 2)
                        ii += 1
                        S.op("pe", lambda: nc.tensor.matmul(ip[:, 0:n], iq[pb:pb + 64, hh // 2, :], ikf[pb:pb + 64, s0:s0 + n], start=True, stop=True),
                             reads=[iqk, "ds_ik"], writes=[ik_])
                        S.op("act", lambda: nc.scalar.activation(r[:, 0:n], ip[:, 0:n], AF.Relu, scale=aw[:, hh:hh + 1]), reads=[ik_, "ds_aw"], writes=[rk])
                        if hh == 0:
                            S.op("dve", lambda: nc.vector.tensor_scalar(acc[:, s0:s0 + n], r[:, 0:n], sg[:, 0:1], None, op0=ALU.mult),
                                 reads=[rk, "ds_sg"], writes=["ds_acc"])
                        else:
                            S.op("dve", lambda: nc.vector.scalar_tensor_tensor(acc[:, s0:s0 + n], r[:, 0:n], sg[:, hh:hh + 1], acc[:, s0:s0 + n], op0=ALU.mult, op1=ALU.add),
                                 reads=[rk, "ds_sg", "ds_acc"], writes=["ds_acc"])
                S.op("dve", lambda: nc.vector.tensor_tensor(acc[:, i * 128:ns], acc[:, i * 128:ns], g.negm[:], op=ALU.add), reads=["ds_acc", "negm"], writes=["ds_acc"])
                src = acc
                for rnd in range(32):
                    S.op("dve", lambda: nc.vector.max(out=m8[:], in_=src[:, 0:ns]), reads=["ds_acc", "ds_wk"], writes=["ds_m8"])
                    if rnd < 31:
                        S.op("dve", lambda: nc.vector.match_replace(out=wk[:, 0:ns], in_to_replace=m8[:], in_values=src[:, 0:ns], imm_value=NEG),
                             reads=["ds_acc", "ds_wk", "ds_m8"], writes=["ds_wk"])
                        src = wk
                S.op("dve", lambda: nc.vector.tensor_scalar(Mq[:, 0:ns], acc[:, 0:ns], m8[:, 7:8], None, op0=ALU.is_ge), reads=["ds_acc", "ds_m8"], writes=["ds_Mq"])
            else:
                if i == 1:
                    S.op("dve", lambda: nc.vector.memset(Mq[:, 0:128], 1.0), writes=["ds_Mq"])
                S.op("dve", lambda: nc.vector.tensor_copy(Mq[:, i * 128:(i + 1) * 128], g.mk_ge[:]), reads=["mk_ge"], writes=["ds_Mq"])
            build_maskT(g, Mq, "ds_Mq", nkt, MT, "ds_MT", trp, "ds_trp")
            for hg in range(2):
                for kt in range(nkt):
                    sp_ = sps[it % 2]
                    sk = "ds_sps%d" % (it % 2)
                    pt = pts[it % 2]
                    pk = "ds_pt%d" % (it % 2)
                    it += 1
                    S.op("pe", lambda: nc.tensor.matmul(sp_[:].rearrange("p (a b) -> p a b", b=128), Kd[:, kt * 128:(kt + 1) * 128], Qd[:, hg * 4:(hg + 1) * 4, i * 128:(i + 1) * 128],
                                                       start=True, stop=True), reads=["ds_K", "ds_Q"], writes=[sk])
                    S.op("act", lambda: nc.scalar.activation(pt[:], sp_[:], AF.Exp, scale=scale), reads=[sk], writes=[pk])
                    S.op("dve", lambda: nc.vector.tensor_tensor(pt[:].rearrange("p (a b) -> p a b", b=128), pt[:].rearrange("p (a b) -> p a b", b=128),
                                                                MT[:, kt:kt + 1, :].to_broadcast([128, 4, 128]), op=ALU.mult), reads=[pk, "ds_MT"], writes=[pk])
                    S.op("pe", lambda: nc.tensor.matmul(ops_[:], Vd[:, kt, :], pt[:], start=(kt == 0), stop=(kt == nkt - 1)), reads=["ds_V", pk], writes=["ds_ops"], sig=False)
                    S.op("pe", lambda: nc.tensor.matmul(dps[:], g.ones[:], pt[:], start=(kt == 0), stop=(kt == nkt - 1)), reads=["ones", pk], writes=["ds_dps"])
                o = ob[oi % 2]
                ok = "ds_ob%d" % (oi % 2)
                oi += 1
                S.op("dve", lambda: nc.vector.reciprocal(rd[:], dps[:]), reads=["ds_dps"], writes=["ds_rd"])
                S.op("dve", lambda: nc.vector.tensor_tensor(o[:].rearrange("p a b -> p (a b)"), ops_[:], rd[:], op=ALU.mult), reads=["ds_ops", "ds_rd"], writes=[ok])
                r0 = 2560 + hg * 512
                S.dma("sp", R["OT"][r0:r0 + 512, i * 128:(i + 1) * 128].rearrange("(a p) t -> p a t", p=128), o[:], reads=[ok], writes=["OT"], nowaw=True)
        S.barrier()


def ph_dil(g, l):
    nc, S, I, R = g.nc, g.S, g.I, g.R
    scale = float(128 ** -0.5)
    GROUPS = ((128, 1), (512, 4), (2048, 16))
    with ExitStack() as st:
        rope = Rope(g, st, 0, "ra")
        xin = g.sbt(st, "dl_x", [128, T])
        xr = g.sbt(st, "dl_xr", [128, T])
        qb = g.sbt(st, "dl_qb", [128, T], BF16)
        kb = g.sbt(st, "dl_kb", [128, T], BF16)
        Vc = g.sbt(st, "dl_V", [128, NT, 128], BF16)
        Oacc = g.sbt(st, "dl_O", [128, T])
        Dacc = g.sbt(st, "dl_D", [128, T])
        m2 = g.sbt(st, "dl_m2", [128, 256], BF16)
        sps = [g.pst(st, "dl_sps%d" % i, [128, 512]) for i in range(2)]
        ops_ = [g.pst(st, "dl_ops%d" % i, [128, 512]) for i in range(2)]
        dps = [g.pst(st, "dl_dps%d" % i, [128, 512]) for i in range(2)]
        pts = [g.sbt(st, "dl_pt%d" % i, [128, 256], BF16) for i in range(2)]
        ob = g.sbt(st, "dl_ob", [128, T], BF16)
        S.op("dve", lambda: nc.vector.tensor_copy(m2[:, 0:128], g.mk_le[:]), reads=["mk_le"], writes=["dl_m2"])
        S.op("dve", lambda: nc.vector.tensor_copy(m2[:, 128:256], g.mk_ge[:]), reads=["mk_ge"], writes=["dl_m2"])
        it = 0
        for u in range(4):
            for gi, (window, d) in enumerate(GROUPS):
                hidx = gi * 4 + u
                nb = T // (128 * d)
                for (nm, dstb, bk) in (("a_q", qb, "dl_qb"), ("a_k", kb, "dl_kb")):
                    r0 = YF_ROWS[nm] + hidx * 128
                    S.dma("sp", xin[:], R["YF"][r0:r0 + 128, :], reads=["YF"], writes=["dl_x"])
                    for tt in range(T // 512):
                        rope.apply(xin[:, tt * 512:(tt + 1) * 512], "dl_x", tt * 512, dstb[:, tt * 512:(tt + 1) * 512], bk)
                vsrc = R["VA"][:, hidx * 128:(hidx + 1) * 128].rearrange("(j i r) c -> i r j c", i=128, r=d)
                for r in range(d):
                    S.dma("sp", Vc[:, r * nb:(r + 1) * nb, :], vsrc[:, r, :, :], reads=["VA"], writes=["dl_V"], nowaw=(r > 0))
                for r in range(d):
                    for j in range(nb):
                        def cols(jj):
                            a0 = r + d * 128 * jj
                            return slice(a0, a0 + d * 127 + 1, d)
                        qs = cols(j)
                        n = 256 if j > 0 else 128
                        sp_ = sps[it % 2]
                        sk = "dl_sps%d" % (it % 2)
                        pt = pts[it % 2]
                        pk = "dl_pt%d" % (it % 2)
                        op_ = ops_[it % 2]
                        okk = "dl_ops%d" % (it % 2)
                        dp_ = dps[it % 2]
                        dk = "dl_dps%d" % (it % 2)
                        it += 1
                        S.op("pe", lambda: nc.tensor.matmul(sp_[:, 0:128], kb[:, qs], qb[:, qs], start=True, stop=True), reads=["dl_kb", "dl_qb"], writes=[sk], sig=(j == 0))
                        if j > 0:
                            S.op("pe", lambda: nc.tensor.matmul(sp_[:, 128:256], kb[:, cols(j - 1)], qb[:, qs], start=True, stop=True), reads=["dl_kb", "dl_qb"], writes=[sk])
                        S.op("act", lambda: nc.scalar.activation(pt[:, 0:n], sp_[:, 0:n], AF.Exp, scale=scale), reads=[sk], writes=[pk])
                        S.op("dve", lambda: nc.vector.tensor_tensor(pt[:, 0:n], pt[:, 0:n], m2[:, 0:n], op=ALU.mult), reads=[pk, "dl_m2"], writes=[pk])
                        S.op("pe", lambda: nc.tensor.matmul(op_[:, 0:128], Vc[:, r * nb + j, :], pt[:, 0:128], start=True, stop=(j == 0)), reads=["dl_V", pk], writes=[okk], sig=False)
                        if j > 0:
                            S.op("pe", lambda: nc.tensor.matmul(op_[:, 0:128], Vc[:, r * nb + j - 1, :], pt[:, 128:256], start=False, stop=True), reads=["dl_V", pk], writes=[okk], sig=False)
                        S.op("pe", lambda: nc.tensor.matmul(dp_[:, 0:128], g.ones[:], pt[:, 0:128], start=True, stop=(j == 0)), reads=["ones", pk], writes=[dk], sig=(j == 0))
                        if j > 0:
                            S.op("pe", lambda: nc.tensor.matmul(dp_[:, 0:128], g.ones[:], pt[:, 128:256], start=False, stop=True), reads=["ones", pk], writes=[dk])
                        if gi == 0:
                            S.op("dve", lambda: nc.vector.tensor_copy(Oacc[:, qs], op_[:, 0:128]), reads=[okk], writes=["dl_O"])
                            S.op("dve", lambda: nc.vector.tensor_copy(Dacc[:, qs], dp_[:, 0:128]), reads=[dk], writes=["dl_D"])
                        else:
                            S.op("dve", lambda: nc.vector.tensor_tensor(Oacc[:, qs], Oacc[:, qs], op_[:, 0:128], op=ALU.add), reads=[okk, "dl_O"], writes=["dl_O"])
                            S.op("dve", lambda: nc.vector.tensor_tensor(Dacc[:, qs], Dacc[:, qs], dp_[:, 0:128], op=ALU.add), reads=[dk, "dl_D"], writes=["dl_D"])
            S.op("dve", lambda: nc.vector.reciprocal(Dacc[:], Dacc[:]), reads=["dl_D"], writes=["dl_D"])
            S.op("dve", lambda: nc.vector.tensor_tensor(ob[:], Oacc[:], Dacc[:], op=ALU.mult), reads=["dl_O", "dl_D"], writes=["dl_ob"])
            S.dma("sp", R["OT"][u * 128:(u + 1) * 128, :], ob[:], reads=["dl_ob"], writes=["OT"], nowaw=True)
        S.barrier()


def ph_branch(g, l):
    nc, S, I, R = g.nc, g.S, g.I, g.R
    L = str(l)
    TB = 2048
    NK = 28
    br = ((0, 4), (4, 12), (12, 20), (20, 28))
    Wb = I["w_branch_" + L]
    Gv = R["G"].rearrange("(b f) t -> f b t", b=4)
    for tb in range(T // TB):
        with ExitStack() as st:
            oT = g.sbt(st, "br_oT", [128, NK, TB], BF16)
            for k0 in range(0, NK, 7):
                S.dma("sp", oT[:, k0:k0 + 7, :], R["OT"][k0 * 128:(k0 + 7) * 128, tb * TB:(tb + 1) * TB].rearrange("(kc p) t -> p kc t", p=128),
                      reads=["OT"], writes=["br_oT"], nowaw=(k0 > 0))
            wb = [g.sbt(st, "br_w%d" % i, [128, NK, 128], BF16) for i in range(2)]
            gt = [g.sbt(st, "br_g%d" % i, [128, 4, 512]) for i in range(2)]
            ps = [g.pst(st, "br_ps%d" % i, [128, 512]) for i in range(8)]
            tmp = [g.sbt(st, "br_t%d" % i, [128, 512]) for i in range(4)]
            yb = [g.sbt(st, "br_y%d" % i, [128, 512], BF16) for i in range(2)]
            it = 0
            for c in range(KC):
                w = wb[c % 2]
                wk = "br_w%d" % (c % 2)
                S.dma("pool", w[:], Wb[:, c * 128:(c + 1) * 128].rearrange("(kc p) n -> p kc n", p=128), writes=[wk])
                for tt in range(TB // 512):
                    t0 = tb * TB + tt * 512
                    gg = gt[it % 2]
                    gk = "br_g%d" % (it % 2)
                    y = yb[it % 2]
                    yk = "br_y%d" % (it % 2)
                    pb = (it % 2) * 4
                    it += 1
                    S.dma("sp", gg[:], Gv[c * 128:(c + 1) * 128, :, t0:t0 + 512], reads=["G"], writes=[gk])
                    for b, (k0, k1) in enumerate(br):
                        for kc in range(k0, k1):
                            S.op("pe", lambda: nc.tensor.matmul(ps[pb + b][:], w[:, kc, :], oT[:, kc, tt * 512:(tt + 1) * 512], start=(kc == k0), stop=(kc == k1 - 1)),
                                 reads=[wk, "br_oT"], writes=["br_ps%d" % (pb + b)], sig=(kc == k1 - 1))
                    for b in range(4):
                        S.op("dve", lambda: nc.vector.tensor_tensor(tmp[b][:], ps[pb + b][:], gg[:, b, :], op=ALU.mult), reads=["br_ps%d" % (pb + b), gk], writes=["br_t%d" % b])
                    S.op("pool", lambda: nc.gpsimd.tensor_tensor(tmp[0][:], tmp[0][:], tmp[1][:], op=ALU.add), reads=["br_t0", "br_t1"], writes=["br_t0"])
                    S.op("pool", lambda: nc.gpsimd.tensor_tensor(tmp[2][:], tmp[2][:], tmp[3][:], op=ALU.add), reads=["br_t2", "br_t3"], writes=["br_t2"])
                    S.op("pool", lambda: nc.gpsimd.tensor_tensor(y[:], tmp[0][:], tmp[2][:], op=ALU.add), reads=["br_t0", "br_t2"], writes=[yk])
                    S.dma("sp", R["YT"][c * 128:(c + 1) * 128, t0:t0 + 512], y[:], reads=[yk], writes=["YT"], nowaw=True)
            S.barrier()


def resid_epi(g, st, tag, xin, xout, xout_key, gate_ap, tok0):
    nc, S = g.nc, g.S
    xo = [g.sbt(st, "%s_xo%d" % (tag, i), [128, 512]) for i in range(3)]
    cnt = [0]

    def epi(c, ci, tt, p, pkey):
        i = cnt[0] % 3
        cnt[0] += 1
        b, k = xo[i], "%s_xo%d" % (tag, i)
        t0 = tok0 + tt * 512
        S.dma("sp", b[:], xin[c * 128:(c + 1) * 128, t0:t0 + 512], reads=["xin_" + tag], writes=[k])
        S.op("dve", lambda: nc.vector.scalar_tensor_tensor(b[:], p, gate_ap[:, c:c + 1], b[:], op0=ALU.mult, op1=ALU.add), reads=[pkey, k], writes=[k])
        S.dma("sp", xout[c * 128:(c + 1) * 128, t0:t0 + 512], b[:], reads=[k], writes=[xout_key], nowaw=True)
    return epi


def ph_wout(g, l, xin, xout):
    nc, S, I, R = g.nc, g.S, g.I, g.R
    L = str(l)
    TB = 2048
    Wo = I["w_out_" + L]
    chunks = [(Wo[:, c * 128:(c + 1) * 128], 128, c) for c in range(KC)]
    for tb in range(T // TB):
        with ExitStack() as st:
            yT = g.sbt(st, "wo_yT", [128, KC, TB], BF16)
            for k0 in range(0, KC, 8):
                S.dma("sp", yT[:, k0:k0 + 8, :], R["YT"][k0 * 128:(k0 + 8) * 128, tb * TB:(tb + 1) * TB].rearrange("(kc p) t -> p kc t", p=128),
                      reads=["YT"], writes=["wo_yT"], nowaw=(k0 > 0))
            epi = resid_epi(g, st, "wo", xin, xout, "xres%d" % (2 * l + 1), g.mod[l][:, 2 * KC:3 * KC], tb * TB)
            proj_fm(g, st, chunks, yT, "wo_yT", KC, TB, epi, tag="pwo")
            S.barrier()


def ph_ffn(g, l, xin, xout):
    nc, S, I, R = g.nc, g.S, g.I, g.R
    L = str(l)
    TB = 2048
    moe = (l % 2 == 1)
    if not moe:
        NF = D_FF // 128
        wg_of = lambda c: I["ffn_gate_0"][:, c * 128:(c + 1) * 128]
        wu_of = lambda c: I["ffn_up_0"][:, c * 128:(c + 1) * 128]
        Wd = I["ffn_down_0"]
    else:
        NF = N_EXP * D_FFE // 128
        CPE = D_FFE // 128
        wg_of = lambda c: I["expert_gate_1"][c // CPE][:, (c % CPE) * 128:(c % CPE + 1) * 128]
        wu_of = lambda c: I["expert_up_1"][c // CPE][:, (c % CPE) * 128:(c % CPE + 1) * 128]
        Wd = I["expert_down_1"]
    for tb in range(T // TB):
        with ExitStack() as st:
            hT = g.sbt(st, "ff_hT", [128, KC, TB], BF16)
            with ExitStack() as st2:
                extra = None
                if moe:
                    rt = g.sbt(st2, "ff_rt", [128, KC, N_EXP])
                    S.dma("sp", rt[:], I["router_1"].rearrange("(kc p) e -> p kc e", p=128), writes=["ff_rt"])
                    lps = g.pst(st2, "ff_lps", [128, 512])
                    tps = g.pst(st2, "ff_tps", [128, 512])
                    lg = g.sbt(st2, "ff_lg", [128, 8])
                    m8 = g.sbt(st2, "ff_m8", [128, 8])
                    nm1 = g.sbt(st2, "ff_nm1", [128, 1])
                    sel = g.sbt(st2, "ff_sel", [128, 8])
                    ew = g.sbt(st2, "ff_ew", [128, 8])
                    den = g.sbt(st2, "ff_den", [128, 1])
                    rwT = g.sbt(st2, "ff_rwT", [8, 128])

                    def extra(ti, hf, hfk):
                        t0 = tb * TB + ti * 128
                        for kc in range(KC):
                            S.op("pe", lambda: nc.tensor.matmul(lps[:, 0:8], hf[:, kc, :], rt[:, kc, :], start=(kc == 0), stop=(kc == KC - 1)),
                                 reads=[hfk, "ff_rt"], writes=["ff_lps"], sig=(kc == KC - 1))
                        S.op("dve", lambda: nc.vector.tensor_copy(lg[:], lps[:, 0:8]), reads=["ff_lps"], writes=["ff_lg"])
                        S.op("dve", lambda: nc.vector.max(out=m8[:], in_=lg[:]), reads=["ff_lg"], writes=["ff_m8"])
                        S.op("dve", lambda: nc.vector.tensor_scalar(nm1[:], m8[:, 0:1], -1.0, None, op0=ALU.mult), reads=["ff_m8"], writes=["ff_nm1"])
                        S.op("dve", lambda: nc.vector.tensor_scalar(sel[:], lg[:], m8[:, 1:2], None, op0=ALU.is_ge), reads=["ff_lg", "ff_m8"], writes=["ff_sel"])
                        S.op("act", lambda: nc.scalar.activation(ew[:], lg[:], AF.Exp, bias=nm1[:, 0:1], scale=1.0), reads=["ff_lg", "ff_nm1"], writes=["ff_ew"])
                        S.op("dve", lambda: nc.vector.tensor_tensor(ew[:], ew[:], sel[:], op=ALU.mult), reads=["ff_ew", "ff_sel"], writes=["ff_ew"])
                        S.op("dve", lambda: nc.vector.tensor_reduce(out=den[:], in_=ew[:], axis=AX.X, op=ALU.add), reads=["ff_ew"], writes=["ff_den"])
                        S.op("dve", lambda: nc.vector.reciprocal(den[:], den[:]), reads=["ff_den"], writes=["ff_den"])
                        S.op("dve", lambda: nc.vector.tensor_scalar(ew[:], ew[:], den[:, 0:1], None, op0=ALU.mult), reads=["ff_ew", "ff_den"], writes=["ff_ew"])
                        S.op("pe", lambda: nc.tensor.transpose(tps[0:8, 0:128], ew[:], g.identf[:]), reads=["ff_ew", "identf"], writes=["ff_tps"])
                        S.op("act", lambda: nc.scalar.copy(rwT[:], tps[0:8, 0:128]), reads=["ff_tps"], writes=["ff_rwT"])
                        S.dma("sp", R["RW"][:, t0:t0 + 128], rwT[:], reads=["ff_rwT"], writes=["RW"], nowaw=True)

                norm_block(g, st2, xin, tb * TB, TB, g.AB[l][:, KC:2 * KC], g.mod[l][:, 3 * KC:4 * KC], hT, "ff_hT", extra=extra, tag="nf")
                S.barrier()
            with ExitStack() as st2:
                wg = [g.sbt(st2, "ff_wg%d" % i, [128, KC, 128], BF16) for i in range(2)]
                wu = [g.sbt(st2, "ff_wu%d" % i, [128, KC, 128], BF16) for i in range(2)]
                pg = [g.pst(st2, "ff_pg%d" % i, [128, 512]) for i in range(2)]
                pu = [g.pst(st2, "ff_pu%d" % i, [128, 512]) for i in range(2)]
                sgb = [g.sbt(st2, "ff_sg%d" % i, [128, 512]) for i in range(2)]
                hb = [g.sbt(st2, "ff_hb%d" % i, [128, 512], BF16) for i in range(3)]
                hf32 = [g.sbt(st2, "ff_hf%d" % i, [128, 512]) for i in range(2)]
                rwb = g.sbt(st2, "ff_rwb", [128, TB]) if moe else None
                it = 0
                for c in range(NF):
                    bi = c % 2
                    S.dma("pool", wg[bi][:], wg_of(c).rearrange("(kc p) n -> p kc n", p=128), writes=["ff_wg%d" % bi])
                    S.dma("pool", wu[bi][:], wu_of(c).rearrange("(kc p) n -> p kc n", p=128), writes=["ff_wu%d" % bi])
                    if moe and c % CPE == 0:
                        e = c // CPE
                        S.dma("sp", rwb[:], R["RW"][e:e + 1, tb * TB:(tb + 1) * TB].to_broadcast([128, TB]), reads=["RW"], writes=["ff_rwb"])
                    for tt in range(TB // 512):
                        i2 = it % 2
                        i3 = it % 3
                        it += 1
                        for kc in range(KC):
                            S.op("pe", lambda: nc.tensor.matmul(pg[i2][:], wg[bi][:, kc, :], hT[:, kc, tt * 512:(tt + 1) * 512], start=(kc == 0), stop=(kc == KC - 1)),
                                 reads=["ff_wg%d" % bi, "ff_hT"], writes=["ff_pg%d" % i2], sig=(kc == KC - 1))
                        for kc in range(KC):
                            S.op("pe", lambda: nc.tensor.matmul(pu[i2][:], wu[bi][:, kc, :], hT[:, kc, tt * 512:(tt + 1) * 512], start=(kc == 0), stop=(kc == KC - 1)),
                                 reads=["ff_wu%d" % bi, "ff_hT"], writes=["ff_pu%d" % i2], sig=(kc == KC - 1))
                        S.op("act", lambda: nc.scalar.activation(sgb[i2][:], pg[i2][:], AF.Silu), reads=["ff_pg%d" % i2], writes=["ff_sg%d" % i2])
                        if not moe:
                            S.op("dve", lambda: nc.vector.tensor_tensor(hb[i3][:], sgb[i2][:], pu[i2][:], op=ALU.mult), reads=["ff_sg%d" % i2, "ff_pu%d" % i2], writes=["ff_hb%d" % i3])
                        else:
                            S.op("dve", lambda: nc.vector.tensor_tensor(hf32[i2][:], sgb[i2][:], pu[i2][:], op=ALU.mult), reads=["ff_sg%d" % i2, "ff_pu%d" % i2], writes=["ff_hf%d" % i2])
                            S.op("pool", lambda: nc.gpsimd.tensor_tensor(hb[i3][:], hf32[i2][:], rwb[:, tt * 512:(tt + 1) * 512], op=ALU.mult),
                                 reads=["ff_hf%d" % i2, "ff_rwb"], writes=["ff_hb%d" % i3])
                        t0 = tb * TB + tt * 512
                        S.dma("sp", R["HID"][c * 128:(c + 1) * 128, t0:t0 + 512], hb[i3][:], reads=["ff_hb%d" % i3], writes=["HID"], nowaw=True)
                S.barrier()
    FO = 512
    KG = 16
    sweeps = [(0, NF)] if NF <= 112 else [(0, NF // 2), (NF // 2, NF)]
    Wdv = Wd.rearrange("(kc p) n -> p kc n", p=128)
    Hv = R["HID"].rearrange("(kc p) t -> p kc t", p=128)
    for si, (ka, kb_) in enumerate(sweeps):
        nk = kb_ - ka
        x_src = xin if si == 0 else R["X5"]
        x_dst = xout if si == len(sweeps) - 1 else R["X5"]
        dkey = "xres%d_%d" % (2 * l + 2, si)
        with ExitStack() as st:
            wd = g.sbt(st, "fd_w", [128, nk, FO], BF16)
            hbuf = [g.sbt(st, "fd_h%d" % i, [128, KG, 512], BF16) for i in range(3)]
            ps = [g.pst(st, "fd_ps%d" % i, [128, 512]) for i in range(8)]
            epi = resid_epi(g, st, "fd", x_src, x_dst, dkey, g.mod[l][:, 5 * KC:6 * KC], 0)
            it = 0
            pi = 0
            for fb in range(D // FO):
                for k0 in range(0, nk, KG):
                    for hcol in range(2):
                        S.dma("pool", wd[:, k0:k0 + KG, hcol * 256:(hcol + 1) * 256],
                              Wdv[:, ka + k0:ka + k0 + KG, fb * FO + hcol * 256:fb * FO + (hcol + 1) * 256], writes=["fd_w"], nowaw=(hcol == 1))
                for tt in range(T // 512):
                    pset = (pi % 2) * 4
                    pi += 1
                    for kg in range(nk // KG):
                        hbf = hbuf[it % 3]
                        hk = "fd_h%d" % (it % 3)
                        it += 1
                        S.dma("sp", hbf[:], Hv[:, ka + kg * KG:ka + (kg + 1) * KG, tt * 512:(tt + 1) * 512], reads=["HID"], writes=[hk])
                        for k in range(KG):
                            kc = kg * KG + k
                            for q in range(4):
                                S.op("pe", lambda: nc.tensor.matmul(ps[pset + q][:], wd[:, kc, q * 128:(q + 1) * 128], hbf[:, k, :], start=(kc == 0), stop=(kc == nk - 1)),
                                     reads=["fd_w", hk], writes=["fd_ps%d" % (pset + q)], sig=(k == KG - 1 and q == 3))
                    for q in range(4):
                        epi(fb * 4 + q, 0, tt, ps[pset + q][:], "fd_ps%d" % (pset + q))
            S.barrier()


def ph_final(g, xin, outT):
    nc, S = g.nc, g.S
    with ExitStack() as st:
        norm_block(g, st, xin, 0, T, g.fnorm, None, None, None, tag="nfin", out_dram=outT)
        S.barrier()


_CACHE = {}


def kernel(**inputs):
    named = {
        "x": inputs["x"], "c": inputs["c"], "positions": inputs["positions"], "w_ada": inputs["w_ada"], "b_ada": inputs["b_ada"],
        "ada_table_0": inputs["ada_table_0"], "mix_norm_0": inputs["mix_norm_0"], "w_in_0": inputs["w_in_0"],
        "mla_q_norm_0": inputs["mla_q_norm_0"], "mla_q_up_0": inputs["mla_q_up_0"], "mla_kv_norm_0": inputs["mla_kv_norm_0"],
        "mla_kv_up_0": inputs["mla_kv_up_0"], "w_branch_0": inputs["w_branch_0"], "w_out_0": inputs["w_out_0"],
        "ffn_norm_0": inputs["ffn_norm_0"], "ffn_gate_0": inputs["ffn_gate_0"], "ffn_up_0": inputs["ffn_up_0"],
        "ffn_down_0": inputs["ffn_down_0"],
        "ada_table_1": inputs["ada_table_1"], "mix_norm_1": inputs["mix_norm_1"], "w_in_1": inputs["w_in_1"],
        "mla_q_norm_1": inputs["mla_q_norm_1"], "mla_q_up_1": inputs["mla_q_up_1"], "mla_kv_norm_1": inputs["mla_kv_norm_1"],
        "mla_kv_up_1": inputs["mla_kv_up_1"], "w_branch_1": inputs["w_branch_1"], "w_out_1": inputs["w_out_1"],
        "ffn_norm_1": inputs["ffn_norm_1"], "router_1": inputs["router_1"], "expert_gate_1": inputs["expert_gate_1"],
        "expert_up_1": inputs["expert_up_1"], "expert_down_1": inputs["expert_down_1"], "final_norm": inputs["final_norm"],
    }
    nb = int(np.asarray(named["x"]).shape[0])
    if "nc" not in _CACHE:
        _CACHE["nc"] = build(layers=(0, 1))
    nc, g = _CACHE["nc"]
    sh = prep_shared(named, (0, 1))
    in_maps = []
    for b in range(nb):
        m = dict(sh)
        m.update(prep_core(named, b))
        in_maps.append({k: m[k] for k in g.I})
    res = run_bass_kernel_spmd(nc, in_maps, core_ids=list(range(nb)))
    out = np.stack([np.ascontiguousarray(np.asarray(res.results[b]["outT"], np.float32).T) for b in range(nb)], axis=0)
    return out
```
